# Optimizing a Trainium2 kernel written in Bass

```python
import math
import jax, jax.numpy as jnp
from jax import lax
import numpy as np

D_MODEL = 1024
BATCH = 4
SEQ = 4096
DEPTH = 1

MEM_LEN = 256
MEM_HEADS = 4
MEM_HEAD_DIM = D_MODEL // 16
MEM_WIDTH = MEM_HEADS * MEM_HEAD_DIM
DIFF_HEADS = 4
DIFF_HEAD_DIM = D_MODEL // 16
DIFF_WIDTH = DIFF_HEADS * 2 * DIFF_HEAD_DIM
FOURIER_GROUPS = 4
FOURIER_GROUP_DIM = D_MODEL // 16
FOURIER_WIDTH = FOURIER_GROUPS * FOURIER_GROUP_DIM
D_MIX = DIFF_WIDTH + FOURIER_WIDTH + MEM_WIDTH
IN_COLS = 3 * DIFF_WIDTH + FOURIER_WIDTH + MEM_WIDTH
N_BUCKETS = 32
MAX_DISTANCE = 128
Q_BLOCK = 128
N_GROUPS = 4
EXPERTS_PER_GROUP = 8
N_EXPERTS = N_GROUPS * EXPERTS_PER_GROUP
TOP_K = 2
D_EXPERT = D_MODEL // 4
LN_EPS = 1e-5
DEEPNORM_ALPHA = (2.0 * DEPTH) ** 0.25
DEEPNORM_BETA = (8.0 * DEPTH) ** -0.25

kernel_name = "hymba_diff_fnet_hmoe_encoder"

F32 = jnp.float32


def layer_norm(x, g, b):
    xf = x.astype(F32)
    mu = jnp.mean(xf, axis=-1, keepdims=True)
    xc = xf - mu
    var = jnp.mean(xc * xc, axis=-1, keepdims=True)
    return (xc * lax.rsqrt(var + LN_EPS) * g.astype(F32) + b.astype(F32)).astype(x.dtype)


def t5_bucket(rel):
    nb = N_BUCKETS // 2
    max_exact = nb // 2
    ret = (rel > 0).astype(jnp.int32) * nb
    n = jnp.abs(rel)
    nf = jnp.maximum(n, 1).astype(F32)
    large = max_exact + (jnp.log(nf / max_exact) / math.log(MAX_DISTANCE / max_exact)
                         * (nb - max_exact)).astype(jnp.int32)
    large = jnp.minimum(large, nb - 1)
    return ret + jnp.where(n < max_exact, n, large)


def diff_attention(q, k, v, lam, lam_init, subln_g, rel_bias):
    B, S = q.shape[0], q.shape[1]
    nblk = S // Q_BLOCK
    scale = DIFF_HEAD_DIM ** -0.5
    kf = k.astype(F32)
    vf = v.astype(F32)
    qb = (q.astype(F32) * scale).reshape(B, nblk, Q_BLOCK, DIFF_HEADS, 2, DIFF_HEAD_DIM)
    qb = qb.transpose(1, 0, 2, 3, 4, 5)
    starts = jnp.arange(nblk, dtype=jnp.int32) * Q_BLOCK
    kpos = jnp.arange(S, dtype=jnp.int32)
    table = rel_bias.astype(F32)

    def block(args):
        qblk, start = args
        qpos = start + jnp.arange(Q_BLOCK, dtype=jnp.int32)
        bucket = t5_bucket(kpos[None, :] - qpos[:, None])
        bias = jnp.take(table, bucket, axis=0).transpose(2, 0, 1)
        s = jnp.einsum('bqhcd,bkhcd->bchqk', qblk, kf) + bias[None, None]
        p = jax.nn.softmax(s, axis=-1)
        a = p[:, 0] - lam * p[:, 1]
        return jnp.einsum('bhqk,bkhe->bqhe', a, vf)

    o = lax.map(block, (qb, starts))
    o = o.transpose(1, 0, 2, 3, 4).reshape(B, S, DIFF_HEADS, 2 * DIFF_HEAD_DIM)
    o = o * lax.rsqrt(jnp.mean(o * o, axis=-1, keepdims=True) + LN_EPS) * subln_g.astype(F32)
    o = o * (1.0 - lam_init)
    return o.reshape(B, S, DIFF_WIDTH).astype(q.dtype)


def fourier_mix(u, w_f):
    B, S = u.shape[0], u.shape[1]
    ug = u.astype(F32).reshape(B, S, FOURIER_GROUPS, FOURIER_GROUP_DIM)
    spec = jnp.fft.fft2(ug, axes=(1, 3), norm="ortho").real
    out = jnp.einsum('bsgc,gcd->bsgd', spec, w_f.astype(F32))
    return out.reshape(B, S, FOURIER_WIDTH).astype(u.dtype)


def memory_attention(mq, mem, w_mem_kv):
    B, S = mq.shape[0], mq.shape[1]
    kv = mem @ w_mem_kv
    mk, mv = jnp.split(kv, 2, axis=-1)
    qh = mq.reshape(B, S, MEM_HEADS, MEM_HEAD_DIM).astype(F32) * (MEM_HEAD_DIM ** -0.5)
    kh = mk.reshape(B, -1, MEM_HEADS, MEM_HEAD_DIM).astype(F32)
    vh = mv.reshape(B, -1, MEM_HEADS, MEM_HEAD_DIM).astype(F32)
    p = jax.nn.softmax(jnp.einsum('bshd,bmhd->bhsm', qh, kh), axis=-1)
    o = jnp.einsum('bhsm,bmhd->bshd', p, vh)
    return o.reshape(B, S, MEM_WIDTH).astype(mq.dtype)


def hybrid_mixer(h, mem, w_in, w_mem_kv, w_fourier, lambda_q1, lambda_k1, lambda_q2, lambda_k2,
                 subln_g, w_out, rel_bias, lam_init):
    B, S = h.shape[0], h.shape[1]
    proj = h @ w_in
    q, k, v, f_in, mq = jnp.split(
        proj, [DIFF_WIDTH, 2 * DIFF_WIDTH, 3 * DIFF_WIDTH, 3 * DIFF_WIDTH + FOURIER_WIDTH], axis=-1)
    q = q.reshape(B, S, DIFF_HEADS, 2, DIFF_HEAD_DIM)
    k = k.reshape(B, S, DIFF_HEADS, 2, DIFF_HEAD_DIM)
    v = v.reshape(B, S, DIFF_HEADS, 2 * DIFF_HEAD_DIM)
    lam = (jnp.exp(jnp.sum(lambda_q1.astype(F32) * lambda_k1.astype(F32)))
           - jnp.exp(jnp.sum(lambda_q2.astype(F32) * lambda_k2.astype(F32))) + lam_init)
    o_diff = diff_attention(q, k, v, lam, lam_init, subln_g, rel_bias)
    o_four = fourier_mix(f_in, w_fourier)
    o_mem = memory_attention(mq, mem, w_mem_kv)
    o = jnp.concatenate([o_diff, o_four, o_mem], axis=-1)
    return o @ w_out


def hier_moe(h, w_group, b_group, w_router, b_router, w1, w3, w2):
    B, S, D = h.shape
    N = B * S
    t = h.reshape(N, D)
    tf = t.astype(F32)
    g_logits = tf @ w_group.astype(F32) + b_group.astype(F32)
    g_prob = jax.nn.softmax(g_logits, axis=-1)
    g_sel = jnp.argmax(g_logits, axis=-1)
    g_gate = jnp.take_along_axis(g_prob, g_sel[:, None], axis=-1)
    e_logits = (tf @ w_router.astype(F32) + b_router.astype(F32)).reshape(N, N_GROUPS, EXPERTS_PER_GROUP)
    e_in_group = jnp.take_along_axis(e_logits, g_sel[:, None, None], axis=1)[:, 0]
    top_val, top_idx = lax.top_k(e_in_group, TOP_K)
    top_w = jax.nn.softmax(top_val, axis=-1) * g_gate
    expert_id = g_sel[:, None] * EXPERTS_PER_GROUP + top_idx
    gates = jnp.sum(jax.nn.one_hot(expert_id, N_EXPERTS, dtype=F32) * top_w[..., None], axis=1)
    out = jnp.zeros((N, D), F32)
    for e in range(N_EXPERTS):
        hid = jax.nn.silu(t @ w1[e]) * (t @ w3[e])
        out = out + gates[:, e:e + 1] * (hid @ w2[e]).astype(F32)
    return out.reshape(B, S, D).astype(h.dtype)


def setup_inputs(seed: int = 0) -> dict:
    key = jax.random.key(seed)
    ks = jax.random.split(key, 32)

    def nrm(k, shape, s):
        return jax.random.normal(k, shape, jnp.float32) * s

    D = D_MODEL
    x = nrm(ks[0], (BATCH, SEQ, D), 1.0)
    mem = nrm(ks[1], (BATCH, MEM_LEN, D), 1.0)
    ln0_g = 1.0 + nrm(ks[2], (D,), 0.02)
    ln0_b = nrm(ks[3], (D,), 0.02)
    rel_bias = nrm(ks[4], (N_BUCKETS, DIFF_HEADS), 0.5)
    col_scale = jnp.concatenate([
        jnp.ones((2 * DIFF_WIDTH,), jnp.float32),
        jnp.full((DIFF_WIDTH,), DEEPNORM_BETA, jnp.float32),
        jnp.ones((FOURIER_WIDTH + MEM_WIDTH,), jnp.float32)])
    w_in = nrm(ks[5], (DEPTH, D, IN_COLS), D ** -0.5) * col_scale
    mem_scale = jnp.concatenate([jnp.ones((MEM_WIDTH,), jnp.float32),
                                 jnp.full((MEM_WIDTH,), DEEPNORM_BETA, jnp.float32)])
    w_mem_kv = nrm(ks[6], (DEPTH, D, 2 * MEM_WIDTH), D ** -0.5) * mem_scale
    w_fourier = nrm(ks[7], (DEPTH, FOURIER_GROUPS, FOURIER_GROUP_DIM, FOURIER_GROUP_DIM), FOURIER_GROUP_DIM ** -0.5)
    lambda_q1 = nrm(ks[8], (DEPTH, DIFF_HEAD_DIM), 0.1)
    lambda_k1 = nrm(ks[9], (DEPTH, DIFF_HEAD_DIM), 0.1)
    lambda_q2 = nrm(ks[10], (DEPTH, DIFF_HEAD_DIM), 0.1)
    lambda_k2 = nrm(ks[11], (DEPTH, DIFF_HEAD_DIM), 0.1)
    subln_g = 1.0 + nrm(ks[12], (DEPTH, 2 * DIFF_HEAD_DIM), 0.02)
    w_out = nrm(ks[13], (DEPTH, D_MIX, D), D_MIX ** -0.5 * DEEPNORM_BETA)
    ln1_g = 1.0 + nrm(ks[14], (DEPTH, D), 0.02)
    ln1_b = nrm(ks[15], (DEPTH, D), 0.02)
    w_group = nrm(ks[16], (DEPTH, D, N_GROUPS), D ** -0.5)
    b_group = nrm(ks[17], (DEPTH, N_GROUPS), 0.01)
    w_router = nrm(ks[18], (DEPTH, D, N_EXPERTS), D ** -0.5)
    b_router = nrm(ks[19], (DEPTH, N_EXPERTS), 0.01)
    w1 = nrm(ks[20], (DEPTH, N_EXPERTS, D, D_EXPERT), D ** -0.5)
    w3 = nrm(ks[21], (DEPTH, N_EXPERTS, D, D_EXPERT), D ** -0.5)
    w2 = nrm(ks[22], (DEPTH, N_EXPERTS, D_EXPERT, D), D_EXPERT ** -0.5 * DEEPNORM_BETA)
    ln2_g = 1.0 + nrm(ks[23], (DEPTH, D), 0.02)
    ln2_b = nrm(ks[24], (DEPTH, D), 0.02)
    return {"x": x, "mem": mem, "ln0_g": ln0_g, "ln0_b": ln0_b, "rel_bias": rel_bias,
            "w_in": w_in, "w_mem_kv": w_mem_kv, "w_fourier": w_fourier,
            "lambda_q1": lambda_q1, "lambda_k1": lambda_k1, "lambda_q2": lambda_q2, "lambda_k2": lambda_k2,
            "subln_g": subln_g, "w_out": w_out, "ln1_g": ln1_g, "ln1_b": ln1_b,
            "w_group": w_group, "b_group": b_group, "w_router": w_router, "b_router": b_router,
            "w1": w1, "w3": w3, "w2": w2, "ln2_g": ln2_g, "ln2_b": ln2_b}


def reference(x, mem, ln0_g, ln0_b, rel_bias, w_in, w_mem_kv, w_fourier,
              lambda_q1, lambda_k1, lambda_q2, lambda_k2, subln_g, w_out, ln1_g, ln1_b,
              w_group, b_group, w_router, b_router, w1, w3, w2, ln2_g, ln2_b):
    h = layer_norm(x, ln0_g, ln0_b)
    for l in range(DEPTH):
        lam_init = 0.8 - 0.6 * math.exp(-0.3 * l)
        a = hybrid_mixer(h, mem, w_in[l], w_mem_kv[l], w_fourier[l],
                         lambda_q1[l], lambda_k1[l], lambda_q2[l], lambda_k2[l],
                         subln_g[l], w_out[l], rel_bias, lam_init)
        h = layer_norm(DEEPNORM_ALPHA * h + a, ln1_g[l], ln1_b[l])
        f = hier_moe(h, w_group[l], b_group[l], w_router[l], b_router[l], w1[l], w3[l], w2[l])
        h = layer_norm(DEEPNORM_ALPHA * h + f, ln2_g[l], ln2_b[l])
    return h
```

```python
import math
import numpy as np
import ml_dtypes
import concourse.bass as bass
import concourse.mybir as mybir
from concourse.bass_utils import run_bass_kernel_spmd

F32 = mybir.dt.float32
BF16 = mybir.dt.bfloat16
U8 = mybir.dt.uint8
I32 = mybir.dt.int32
U32 = mybir.dt.uint32
AF = mybir.ActivationFunctionType
ALU = mybir.AluOpType
AX = mybir.AxisListType

S = 4096
OWN = 2048
D = 1024
NE = 32
ALPHA = 2.0 ** 0.25
EPS = 1e-5
LAM_INIT = 0.2
GW = 1280
HW = 1152
SBUF_BYTES = 207 * 1024


class Prog:
    BK = 2048

    def __init__(self, nc):
        self.nc = nc
        self.ops = []
        self.lanes = {}
        self.bk = {}

    def _buckets(self, sp, lo, hi):
        return [(sp, b) for b in range(lo // self.BK, (hi - 1) // self.BK + 1)]

    def add(self, eng, fn, reads=(), writes=(), lane=None, group=False):
        idx = len(self.ops)
        deps = set()
        reads = [(sp, lo // 2048 * 2048, (hi + 2047) // 2048 * 2048) if sp == "ps" else (sp, lo, hi) for (sp, lo, hi) in reads]
        writes = [(sp, lo // 2048 * 2048, (hi + 2047) // 2048 * 2048) if sp == "ps" else (sp, lo, hi) for (sp, lo, hi) in writes]
        for (sp, lo, hi) in reads:
            for key in self._buckets(sp, lo, hi):
                for a in self.bk.get(key, ()):
                    if a[0] < hi and lo < a[1] and (a[3] or (sp == "ps" and self.ops[a[2]]["eng"] != eng)):
                        deps.add(a[2])
        for (sp, lo, hi) in writes:
            for key in self._buckets(sp, lo, hi):
                for a in self.bk.get(key, ()):
                    if a[0] < hi and lo < a[1]:
                        deps.add(a[2])
        deps.discard(idx)
        if lane is not None and group:
            deps = {d for d in deps if self.ops[d]["lane"] != lane}
        op = dict(eng=eng, fn=fn, deps=deps, lane=lane)
        self.ops.append(op)
        for (sp, lo, hi) in writes:
            for key in self._buckets(sp, lo, hi):
                L = self.bk.setdefault(key, [])
                L[:] = [a for a in L if not (lo <= a[0] and a[1] <= hi)]
                L.append([lo, hi, idx, True])
        for (sp, lo, hi) in reads:
            for key in self._buckets(sp, lo, hi):
                L = self.bk.setdefault(key, [])
                if lane is None:
                    L[:] = [a for a in L if not ((not a[3]) and a[0] == lo and a[1] == hi
                                                 and self.ops[a[2]]["eng"] == eng and self.ops[a[2]]["lane"] is None)]
                L.append([lo, hi, idx, False])
        if lane is not None:
            Ln = self.lanes.setdefault(lane, dict(group=group, count=0))
            Ln["count"] += 1
            op["lane_seq"] = Ln["count"]
        return idx

    def emit(self):
        nc = self.nc
        ops = self.ops
        engs = ["pe", "act", "dve", "pool", "sp"]
        for i, op in enumerate(ops):
            best = {}
            keep = set()
            for d in op["deps"]:
                pd = ops[d]
                if pd["lane"] is not None:
                    keep.add(d)
                    continue
                if pd["eng"] == "pe" and op["eng"] == "pe" and op["lane"] is None:
                    continue
                if pd["eng"] not in best or best[pd["eng"]] < d:
                    best[pd["eng"]] = d
            keep.update(best.values())
            op["deps"] = keep
        needed = [False] * len(ops)
        for op in ops:
            for d in op["deps"]:
                needed[d] = True
        sems = {e: nc.alloc_semaphore(name="sem_" + e) for e in engs}
        lane_sems = {l: nc.alloc_semaphore(name="lane_%d" % k) for k, l in enumerate(self.lanes)}
        cnt = {e: 0 for e in engs}
        for i, op in enumerate(ops):
            if op["lane"] is not None:
                Ln = self.lanes[op["lane"]]
                v = 16 * (Ln["count"] if Ln["group"] else op["lane_seq"])
                op["sig"] = (lane_sems[op["lane"]], v)
            elif needed[i]:
                cnt[op["eng"]] += 1
                op["sig"] = (sems[op["eng"]], cnt[op["eng"]])
            else:
                op["sig"] = None
        by_eng = {e: [] for e in engs}
        for i, op in enumerate(ops):
            by_eng[op["eng"]].append(i)
        lanes_by_eng = {e: set() for e in engs}
        for op in ops:
            if op["lane"] is not None:
                lanes_by_eng[op["eng"]].add(op["lane"])
        block_cm = nc.Block()
        block = block_cm.__enter__()

        def run_engine(e, handle):
            waited = {}
            for i in by_eng[e]:
                op = ops[i]
                need = {}
                for d in op["deps"]:
                    s, v = ops[d]["sig"]
                    key = id(s)
                    if key not in need or need[key][1] < v:
                        need[key] = (s, v)
                for key, (s, v) in need.items():
                    if waited.get(key, -1) >= v:
                        continue
                    handle.wait_ge(s, v)
                    waited[key] = v
                inst = op["fn"](handle)
                if op["lane"] is not None:
                    inst.then_inc(op["sig"][0], 16)
                elif op["sig"] is not None:
                    inst.then_inc(op["sig"][0], 1)
            for l in sorted(lanes_by_eng[e]):
                handle.wait_ge(lane_sems[l], 16 * self.lanes[l]["count"])

        @block.tensor
        def _(h):
            run_engine("pe", h)

        @block.scalar
        def _(h):
            run_engine("act", h)

        @block.vector
        def _(h):
            run_engine("dve", h)

        @block.gpsimd
        def _(h):
            run_engine("pool", h)

        @block.sync
        def _(h):
            run_engine("sp", h)

        block_cm.__exit__(None, None, None)


class SB:
    def __init__(self, big, off, shape, dt):
        self.esz = 4 if dt in (F32, I32, U32) else 2
        self.n = int(np.prod(shape[1:]))
        self.off = off
        self.shape = shape
        ap = big[:, off:off + self.n * self.esz].bitcast(dt)
        if len(shape) == 3:
            ap = ap.rearrange("p (a b) -> p a b", a=shape[1])
        elif len(shape) == 4:
            ap = ap.rearrange("p (a b c) -> p a b c", a=shape[1], b=shape[2])
        self.ap = ap

    def iv(self, lo=0, hi=None):
        if hi is None:
            hi = self.n
        return ("sb", self.off + lo * self.esz, self.off + hi * self.esz)

    def ivi(self, *idx):
        stride = self.n
        lo = 0
        for k, i in enumerate(idx):
            stride //= self.shape[1 + k]
            lo += i * stride
        return self.iv(lo, lo + stride)


def PSI(bank, lo=0, hi=512, nb=1):
    return ("ps", bank * 2048 + lo * 4, (bank + nb - 1) * 2048 + hi * 4)


def _t5_bucket(rel):
    nb = 16
    max_exact = 8
    ret = (rel > 0).astype(np.int64) * nb
    n = np.abs(rel)
    nf = np.maximum(n, 1).astype(np.float32)
    large = max_exact + (np.log(nf / np.float32(max_exact)) / np.float32(math.log(128 / max_exact))
                         * np.float32(nb - max_exact)).astype(np.int32)
    large = np.minimum(large, nb - 1)
    return ret + np.where(n < max_exact, n, large)


def _band_kbs(qg):
    return [(kb % 32) for kb in range(4 * qg - 1, 4 * qg + 5)]


def _host_consts(half):
    c = {}
    c["identb"] = np.eye(128, dtype=np.float32).astype(ml_dtypes.bfloat16)
    c["identf"] = np.eye(128, dtype=np.float32)
    c["jb"] = np.eye(128, dtype=np.float32)[::-1].copy().astype(ml_dtypes.bfloat16)
    n = np.arange(GW)
    bk = _t5_bucket(639 - n)
    oh = np.zeros((32, GW), np.float32)
    oh[bk, n] = 1.0
    oh2 = np.zeros((32, GW), np.float32)
    if half == 0:
        oh2[:, :640] = oh[:, :640]
        oh2[31, 640:] = 1.0
    else:
        oh2[15, :640] = 1.0
        oh2[:, 640:] = oh[:, 640:]
    c["oh"] = oh
    c["oh2"] = oh2
    selb = np.zeros((128,), np.float32)
    for qg in range(4):
        band = _band_kbs(qg)
        for kb in range(32):
            if kb in band:
                continue
            if kb < 16:
                before = kb < 4 * qg - 1
            else:
                before = (half == 1)
            selb[qg * 32 + kb] = 1.0 if before else 0.0
    c["selb"] = selb
    selb2 = np.zeros((256,), np.float32)
    for g in range(8):
        band2 = [(kb % 32) for kb in range(2 * g - 1, 2 * g + 3)]
        for kb in range(32):
            if kb in band2:
                continue
            if kb < 16:
                before = kb < 2 * g - 1
            else:
                before = (half == 1)
            selb2[g * 32 + kb] = 1.0 if before else 0.0
    c["selb2"] = selb2
    k = np.arange(64)
    ang = 2.0 * np.pi * ((k[:, None] * k[None, :]) % 64) / 64.0
    c64 = (np.cos(ang) / 512.0).astype(np.float32)
    s64 = (np.sin(ang) / 512.0).astype(np.float32)
    z = np.zeros((64, 64), np.float32)
    c["c64bd"] = np.block([[c64, z], [z, c64]])
    c["s64bd"] = np.block([[s64, z], [z, s64]])
    pt = (np.arange(S) + OWN * half) % S
    ps = np.arange(OWN) + OWN * half
    idx = (pt[:, None] * ps[None, :]) % S
    tab = 2.0 * np.pi * np.arange(S) / S
    ct = np.cos(tab).astype(np.float32).astype(ml_dtypes.bfloat16)
    st = (-np.sin(tab)).astype(np.float32).astype(ml_dtypes.bfloat16)
    dft = np.empty((2, 4, S, 512), ml_dtypes.bfloat16)
    for sg in range(4):
        sub = idx[:, sg * 512:(sg + 1) * 512]
        dft[0, sg] = ct[sub]
        dft[1, sg] = st[sub]
    c["dft"] = dft
    c["u_tri"] = np.triu(np.ones((128, 128), np.float32), 1)
    c["ones_c"] = np.ones((128, 128), np.float32)
    c["tau_c"] = np.arange(64, dtype=np.float32)
    pj = np.zeros((128, 4), np.float32)
    pj[:, 0:3] = 3.0 * np.arange(128, dtype=np.float32)[:, None] + np.arange(3, dtype=np.float32)[None, :]
    pj[:, 3] = np.arange(128, dtype=np.float32)
    c["pj_c"] = pj
    c["thr_c"] = np.tile(128.0 * np.arange(16, dtype=np.float32)[None, :], (32, 1)).reshape(512)
    return c


def build_nc(stop=None):
    nc = bass.Bass("TRN2", target_bir_lowering=False)

    def finish(dumps):
        for name, sbuf in dumps:
            shp = [128, sbuf.n]
            dt = F32 if sbuf.esz == 4 else BF16
            d = nc.dram_tensor("dbg_" + name, shp, dt, kind="ExternalOutput").ap()
            flat = big[:, sbuf.off:sbuf.off + sbuf.n * sbuf.esz].bitcast(dt)
            P.add("sp", lambda e, d=d, flat=flat: e.dma_start(out=d, in_=flat), reads=[sbuf.iv()], writes=[("dbg_" + name, 0, 1)],
                  lane="dbg_" + name)
        P.emit()
        return nc

    def din(name, shape, dt=F32):
        return nc.dram_tensor(name, list(shape), dt, kind="ExternalInput").ap()

    x = din("x", [S, D])
    mem = din("mem", [256, D])
    g0T = din("g0T", [128, 8])
    b0T = din("b0T", [128, 8])
    ln0_g = din("ln0_g", [D])
    ln0_b = din("ln0_b", [D])
    rel_bias = din("rel_bias", [32, 4])
    w_in = din("w_in", [D, 2048])
    w_mem = din("w_mem", [D, 512])
    w_four = din("w_four", [4, 64, 64])
    lq1 = din("lq1", [64])
    lk1 = din("lk1", [64])
    lq2 = din("lq2", [64])
    lk2 = din("lk2", [64])
    subln = din("subln", [128])
    w_out = din("w_out", [D, D])
    ln1_g = din("ln1_g", [D])
    ln1_b = din("ln1_b", [D])
    wr_cat = din("wr_cat", [D, 36])
    b_cat = din("b_cat", [36])
    wp_d = din("wp", [NE * 128 * 3, 2048])
    u_d = din("u_tri", [128, 128])
    ones_d = din("ones_c", [128, 128])
    tau_d = din("tau_c", [64])
    pj_d = din("pj_c", [128, 4])
    thr_d = din("thr_c", [512])
    ln2_g = din("ln2_g", [D])
    ln2_b = din("ln2_b", [D])
    identb_d = din("identb", [128, 128], BF16)
    identf_d = din("identf", [128, 128])
    jb_d = din("jb", [128, 128], BF16)
    oh_d = din("oh", [32, GW])
    oh2_d = din("oh2", [32, GW])
    selb_d = din("selb", [128])
    selb2_d = din("selb2", [256])
    c64_d = din("c64bd", [128, 128])
    s64_d = din("s64bd", [128, 128])
    dft = din("dft", [2, 4, S, 512], BF16)
    gscr = nc.dram_tensor("gscr", [8, GW], F32, kind="Internal").ap()
    xs_z = nc.dram_tensor("xs_scr", [64 * 128, D + 4], BF16, kind="Internal").ap()
    y = nc.dram_tensor("y", [OWN, D], F32, kind="ExternalOutput").ap()

    big = nc.sbuf_tensor("big", [128, SBUF_BYTES], U8).__enter__()
    ps = nc.psum_tensor("ps", [128, 8, 512], F32).__enter__()

    def psb(bank):
        return ps[:, bank, :].bitcast(BF16)

    P = Prog(nc)
    cur = [0]

    def carve(shape, dt, at=None):
        esz = 4 if dt in (F32, I32, U32) else 2
        nbytes = int(np.prod(shape[1:])) * esz
        if at is None:
            off = cur[0]
            cur[0] = (off + nbytes + 63) // 64 * 64
            assert cur[0] <= SBUF_BYTES, ("sbuf overflow", cur[0])
        else:
            off = at
        return SB(big, off, shape, dt)

    kT = carve([128, 4, S], BF16)
    Vaug = carve([128, 32, 4, 130], BF16)
    qT = carve([128, 4, OWN], BF16)
    utok = carve([128, 32, 256], BF16)
    mqT = carve([128, 2, OWN], BF16)
    identb = carve([128, 128], BF16)
    identf = carve([128, 128], F32)
    jb = carve([128, 128], BF16)
    tabB = carve([128, 128], F32)
    selb = carve([128, 128], F32)
    cb = carve([128, 4, 128], F32)
    g0Ts = carve([128, 8], F32)
    b0Ts = carve([128, 8], F32)
    stat0 = carve([128, 16, 2], F32)
    lamb = carve([128, 4, 64], F32)
    small = carve([128, 64], F32)
    gsub = carve([128, 128], F32)
    stt = carve([128, 8, 16], F32)
    mkT = carve([128, 2, 256], BF16)
    mvaug = carve([128, 2, 4, 66], BF16)
    Hb = carve([128, 4, HW], BF16)
    H2b = carve([128, 4, HW], BF16)
    Hst = carve([128, GW], F32)
    ABD = carve([128, 2, 256], BF16)
    tab32 = carve([128, 8], F32)
    tabP = carve([128, 128], F32)
    ohs = carve([128, GW], F32)
    R1 = cur[0]
    cur[0] += 32768
    R2 = cur[0]
    cur[0] += 32768
    assert cur[0] <= SBUF_BYTES, cur[0]
    w_inb = carve([128, 8, 2048], BF16, at=R1)
    h0T = [carve([128, 8, 512], BF16, at=R2 + i * 8192) for i in range(2)]
    xt = [carve([128, D], F32, at=R2 + 16384 + i * 4096) for i in range(2)]
    xn = [carve([128, D], BF16, at=R2 + 24576 + i * 2048) for i in range(4)]
    wmem = carve([128, 8, 512], BF16, at=R2)
    memf = carve([128, 2, D], F32, at=R2 + 8192)
    membf = carve([128, 2, D], BF16, at=R2 + 24576)
    memT = carve([128, 8, 256], BF16, at=R2 + 28672)
    wbd = carve([128, 2, 128], F32, at=R1 + 24576)
    ring = [carve([128, 8, 512], BF16, at=R1 + i * 8192) for i in range(3)]
    ybuf = carve([128, 4, 512], BF16, at=R1 + 24576)
    oT = carve([128, 8, OWN], BF16, at=R2)
    PT2 = [carve([128, 2, 512], BF16, at=R1 + 8192 + i * 2048) for i in range(3)]
    PT = [[carve([128, 512], BF16, at=R1 + 8192 + i * 2048 + c * 1024) for i in range(3)] for c in range(2)]
    accS = [carve([128, 264], F32, at=R1 + 16384 + i * 1088) for i in range(2)]
    post = carve([128, 8, 128], F32, at=R1 + 20480)

    def dma(eng, out_ap, in_ap, reads, writes, lane, group=False):
        P.add(eng, lambda e: e.dma_start(out=out_ap, in_=in_ap), reads=reads, writes=writes, lane=lane, group=group)

    def mm(out_ap, lhsT, rhs, start, stop, reads, writes, skip=False):
        if skip:
            P.add("pe", lambda e: e.matmul(out_ap, lhsT=lhsT, rhs=rhs, start=start, stop=stop, skip_group_check=True),
                  reads=reads, writes=writes)
        else:
            P.add("pe", lambda e: e.matmul(out_ap, lhsT=lhsT, rhs=rhs, start=start, stop=stop), reads=reads, writes=writes)

    def tr(out_ap, in_ap, ident, reads, writes):
        P.add("pe", lambda e: e.transpose(out=out_ap, in_=in_ap, identity=ident), reads=reads, writes=writes)

    def act(out_ap, in_ap, func, reads, writes, bias=None, scale=None, accum=None):
        kw = {}
        if bias is not None:
            kw["bias"] = bias
        if scale is not None:
            kw["scale"] = scale
        if accum is not None:
            kw["accum_out"] = accum
        P.add("act", lambda e: e.activation(out=out_ap, in_=in_ap, func=func, **kw), reads=reads, writes=writes)

    def ts(eng, out_ap, in_ap, s1, s2, op0, op1, reads, writes):
        if op1 is None:
            P.add(eng, lambda e: e.tensor_scalar(out=out_ap, in0=in_ap, scalar1=s1, scalar2=None, op0=op0), reads=reads, writes=writes)
        else:
            P.add(eng, lambda e: e.tensor_scalar(out=out_ap, in0=in_ap, scalar1=s1, scalar2=s2, op0=op0, op1=op1), reads=reads, writes=writes)

    def tt(eng, out_ap, a, b, op, reads, writes):
        P.add(eng, lambda e: e.tensor_tensor(out=out_ap, in0=a, in1=b, op=op), reads=reads, writes=writes)

    def stt_(out_ap, in0, scalar, in1, op0, op1, reads, writes, accum=None):
        if accum is None:
            P.add("dve", lambda e: e.scalar_tensor_tensor(out=out_ap, in0=in0, scalar=scalar, in1=in1, op0=op0, op1=op1),
                  reads=reads, writes=writes)
        else:
            P.add("dve", lambda e: e.scalar_tensor_tensor(out=out_ap, in0=in0, scalar=scalar, in1=in1, op0=op0, op1=op1,
                                                          accum_out=accum), reads=reads, writes=writes)

    def cp(eng, out_ap, in_ap, reads, writes):
        P.add(eng, lambda e: e.tensor_copy(out=out_ap, in_=in_ap), reads=reads, writes=writes)

    def memset(eng, ap, val, writes):
        P.add(eng, lambda e: e.memset(ap, val), writes=writes)

    G = dict(lane="c", group=True)
    G0 = dict(lane="c0", group=True)
    for t_ in range(2):
        dma("sp", xt[t_].ap, x[t_ * 128:(t_ + 1) * 128, :], [], [xt[t_].iv()], lane="x%d" % t_)
    dma("sp", identb.ap, identb_d, [], [identb.iv()], **G0)
    dma("sp", g0Ts.ap, g0T, [], [g0Ts.iv()], **G0)
    dma("sp", b0Ts.ap, b0T, [], [b0Ts.iv()], **G0)
    dma("sp", identf.ap, identf_d, [], [identf.iv()], **G)
    dma("sp", jb.ap, jb_d, [], [jb.iv()], **G)
    dma("sp", tabB.ap, rel_bias.rearrange("a b -> (a b)").partition_broadcast(128), [], [tabB.iv()], **G)
    dma("sp", selb.ap, selb_d.partition_broadcast(128), [], [selb.iv()], **G)
    for i, v in enumerate([lq1, lk1, lq2, lk2]):
        dma("sp", lamb.ap[:, i, :], v.partition_broadcast(128), [], [lamb.ivi(i)], **G)
    dma("sp", gsub.ap, subln.partition_broadcast(128), [], [gsub.iv()], **G)
    dma("sp", tab32.ap[0:32, 0:4], rel_bias, [], [tab32.iv(0, 4)], **G)
    dma("sp", tab32.ap[0:32, 4:8], rel_bias[31, :].partition_broadcast(32), [], [tab32.iv(4, 8)], **G)
    memset("pool", ohs.ap, 0.0, [ohs.iv()])
    memset("pool", tabP.ap, 0.0, [tabP.iv()])
    dma("sp", ohs.ap[0:32, :], oh_d, [], [ohs.iv()], lane="oh1")
    if stop == "c0":
        return finish([("small", small), ("gsub", gsub)])
    w_in_v = w_in.rearrange("(k p) n -> p k n", p=128)
    for cbk in (1, 0, 3, 2):
        c0 = cbk * 512
        dma("pool", w_inb.ap[:, :, c0:c0 + 512], w_in_v[:, :, c0:c0 + 512], [],
            [w_inb.iv(k * 2048 + c0, k * 2048 + c0 + 512) for k in range(8)], lane="w_in%d" % cbk)
    if stop == "c0w":
        return finish([("small", small), ("gsub", gsub)])
    Vaug3 = carve([128, 128, 130], BF16, at=Vaug.off)
    mvaug3 = carve([128, 8, 66], BF16, at=mvaug.off)
    memset("pool", Vaug3.ap[:, :, 128:130], 1.0, [Vaug.iv()])
    memset("pool", mvaug3.ap[:, :, 64:66], 1.0, [mvaug.iv()])
    if stop == "c1":
        return finish([("small", small), ("gsub", gsub)])

    sm = small.ap
    junk = Hst.ap[:, 0:64]
    stt_(junk, lamb.ap[:, 0, :], 1.0, lamb.ap[:, 1, :], ALU.mult, ALU.mult, [lamb.ivi(0), lamb.ivi(1)],
         [Hst.iv(), small.iv(0, 1)], accum=sm[:, 0:1])
    stt_(junk, lamb.ap[:, 2, :], 1.0, lamb.ap[:, 3, :], ALU.mult, ALU.mult, [lamb.ivi(2), lamb.ivi(3)],
         [Hst.iv(), small.iv(1, 2)], accum=sm[:, 1:2])
    act(sm[:, 2:4], sm[:, 0:2], AF.Exp, [small.iv(0, 2)], [small.iv(2, 4)])
    tt("dve", sm[:, 4:5], sm[:, 3:4], sm[:, 2:3], ALU.subtract, [small.iv(2, 4)], [small.iv(4, 5)])
    ts("dve", sm[:, 4:5], sm[:, 4:5], -LAM_INIT, None, ALU.add, None, [small.iv(4, 5)], [small.iv(4, 5)])
    ts("dve", gsub.ap, gsub.ap, 1.0 - LAM_INIT, None, ALU.mult, None, [gsub.iv()], [gsub.iv()])
    tt("dve", tabP.ap[0:32, 0:4], tab32.ap[0:32, 0:4], tab32.ap[0:32, 4:8], ALU.subtract, [tab32.iv(), tabP.iv()], [tabP.iv()])
    for h in range(4):
        tt("dve", sm[:, 8 + h:9 + h], tabB.ap[:, 60 + h:61 + h], tabB.ap[:, 124 + h:125 + h], ALU.subtract,
           [tabB.iv()], [small.iv(8 + h, 9 + h)])
        ts("dve", cb.ap[:, h, :], selb.ap, sm[:, 8 + h:9 + h], None, ALU.mult, None, [selb.iv(), small.iv(8 + h, 9 + h)], [cb.ivi(h)])
    if stop == "const":
        return finish([("Hb", Hb), ("H2b", H2b), ("cb", cb), ("small", small), ("gsub", gsub)])
    rr = [0]

    def evac_eng():
        rr[0] += 1
        return "act" if rr[0] % 2 else "dve"

    def evac(out_ap, in_ap, reads, writes, scale=None, eng=None):
        e = eng or evac_eng()
        if e == "act":
            if scale is None:
                act(out_ap, in_ap, AF.Copy, reads, writes)
            else:
                act(out_ap, in_ap, AF.Copy, reads, writes, scale=scale)
        else:
            if scale is None:
                cp("dve", out_ap, in_ap, reads, writes)
            else:
                ts("dve", out_ap, in_ap, scale, None, ALU.mult, None, reads, writes)

    def ln_stats(src_ap, src_iv, slot, rstd_ap, rstd_iv, nb_ap, nb_iv):
        s = stt.ap[:, slot, :]
        siv = stt.ivi(slot)
        P.add("dve", lambda e: e.bn_stats(out=s[:, 0:6], in_=src_ap[:, 0:512]), reads=[src_iv], writes=[siv])
        P.add("dve", lambda e: e.bn_stats(out=s[:, 6:12], in_=src_ap[:, 512:1024]), reads=[src_iv], writes=[siv])
        P.add("dve", lambda e: e.bn_aggr(out=s[:, 12:14], in_=s[:, 0:12]), reads=[siv], writes=[siv])
        act(s[:, 14:15], s[:, 13:14], AF.Ln, [siv], [siv], bias=EPS, scale=1.0)
        act(rstd_ap, s[:, 14:15], AF.Exp, [siv], [rstd_iv], scale=-0.5)
        stt_(nb_ap, s[:, 12:13], -1.0, rstd_ap, ALU.mult, ALU.mult, [siv, rstd_iv], [nb_iv])

    pbank = [4]

    def next_bank(lo=4, hi=8):
        b = pbank[0]
        pbank[0] = lo + (pbank[0] - lo + 1) % (hi - lo)
        return b

    def ln_tile(t):
        xb_ = xt[t % 2]
        if t >= 2:
            dma("sp", xb_.ap, x[t * 128:(t + 1) * 128, :], [], [xb_.iv()], lane="x%d" % (t % 2))
        slot = t % 8
        if t < 16:
            r_ap, r_iv = stat0.ap[:, t, 0:1], stat0.iv(2 * t, 2 * t + 1)
            n_ap, n_iv = stat0.ap[:, t, 1:2], stat0.iv(2 * t + 1, 2 * t + 2)
        else:
            r_ap, r_iv = stt.ap[:, slot, 14:15], stt.ivi(slot)
            n_ap, n_iv = stt.ap[:, slot, 15:16], stt.ivi(slot)
        ln_stats(xb_.ap, xb_.iv(), slot, r_ap, r_iv, n_ap, n_iv)
        xnb = xn[t % 4]
        act(xnb.ap, xb_.ap, AF.Identity, [xb_.iv(), r_iv, n_iv], [xnb.iv()], bias=n_ap, scale=r_ap)

    def tr_tile(t):
        Gi, tl = t // 4, t % 4
        hb = h0T[Gi % 2]
        xnb = xn[t % 4]
        tb = 2 * (t % 2)
        for k in range(8):
            tr(psb(tb + k // 4)[:, (k % 4) * 128:(k % 4 + 1) * 128], xnb.ap[:, k * 128:(k + 1) * 128], identb.ap,
               [xnb.iv(), identb.iv()], [PSI(tb + k // 4)])
        for k in range(8):
            src = psb(tb + k // 4)[:, (k % 4) * 128:(k % 4 + 1) * 128]
            dst = hb.ap[:, k, tl * 128:(tl + 1) * 128]
            div = hb.iv(k * 512 + tl * 128, k * 512 + tl * 128 + 128)
            if k < 4:
                ts("dve", dst, src, g0Ts.ap[:, k:k + 1], b0Ts.ap[:, k:k + 1], ALU.mult, ALU.add,
                   [PSI(tb), g0Ts.iv(), b0Ts.iv()], [div])
            else:
                act(dst, src, AF.Identity, [PSI(tb + 1), g0Ts.iv(), b0Ts.iv()], [div],
                    bias=b0Ts.ap[:, k:k + 1], scale=g0Ts.ap[:, k:k + 1])

    def proj_units(Gi):
        hb = h0T[Gi % 2]
        c0, c1 = Gi * 512, Gi * 512 + 512
        units = []

        def fm(col0, dst_ap, dst_iv, scale=None):
            def u():
                b = next_bank()
                for k in range(8):
                    mm(ps[:, b, :], w_inb.ap[:, k, col0:col0 + 128], hb.ap[:, k, :], k == 0, k == 7,
                       [w_inb.iv(k * 2048 + col0, k * 2048 + col0 + 128), hb.ivi(k)], [PSI(b)])
                evac(dst_ap, ps[:, b, :], [PSI(b)], [dst_iv], scale=scale)
            return u

        for h in range(4):
            units.append(fm(512 + 128 * h, kT.ap[:, h, c0:c1], kT.iv(h * S + c0, h * S + c1)))
        if Gi < 4:
            for h in range(4):
                units.append(fm(128 * h, qT.ap[:, h, c0:c1], qT.iv(h * OWN + c0, h * OWN + c1), 0.125))
            for j in range(2):
                units.append(fm(1792 + 128 * j, mqT.ap[:, j, c0:c1], mqT.iv(j * OWN + c0, j * OWN + c1), 0.125))
        for tl in range(4):
            t = Gi * 4 + tl

            def uv(t=t, tl=tl):
                b = next_bank()
                for k in range(8):
                    mm(ps[:, b, :], hb.ap[:, k, tl * 128:(tl + 1) * 128], w_inb.ap[:, k, 1024:1536], k == 0, k == 7,
                       [w_inb.iv(k * 2048 + 1024, k * 2048 + 1536), hb.ivi(k)], [PSI(b)])
                evac(Vaug.ap[:, t, :, 0:128], ps[:, b, :].rearrange("p (a b) -> p a b", a=4), [PSI(b)], [Vaug.ivi(t)])

            def uu(t=t, tl=tl):
                b = next_bank()
                for k in range(8):
                    mm(ps[:, b, 0:256], hb.ap[:, k, tl * 128:(tl + 1) * 128], w_inb.ap[:, k, 1536:1792], k == 0, k == 7,
                       [w_inb.iv(k * 2048 + 1536, k * 2048 + 1792), hb.ivi(k)], [PSI(b, 0, 256)])
                evac(utok.ap[:, t, :], ps[:, b, 0:256], [PSI(b, 0, 256)], [utok.ivi(t)])
            units.append(uv)
            units.append(uu)
        return units

    for tl in range(4):
        ln_tile(tl)
    for tl in range(4):
        tr_tile(tl)
    for Gi in range(8):
        units = proj_units(Gi)
        nxt = [(Gi + 1) * 4 + tl for tl in range(4)] if Gi < 7 else []
        for t in nxt:
            ln_tile(t)
        nu = len(units)
        q = (nu + 3) // 4
        for j in range(4):
            for u in units[j * q:(j + 1) * q]:
                u()
            if nxt:
                tr_tile(nxt[j])

    for var, ohd, Hdst in ((0, None, Hb), (1, oh2_d, H2b)):
        if var == 1:
            dma("sp", ohs.ap[0:32, :], ohd, [], [ohs.iv()], lane="oh2")
        for j in range(3):
            n0 = j * 512
            n1 = min(GW, n0 + 512)
            mm(ps[:, j, 0:n1 - n0], tabP.ap, ohs.ap[:, n0:n1], True, True,
               [tabP.iv(), ohs.iv()], [PSI(j)])
            cp("dve", Hst.ap[0:4, n0:n1], ps[0:4, j, 0:n1 - n0], [PSI(j)], [Hst.iv()])
        dma("sp", gscr[var * 4:var * 4 + 4, :], Hst.ap[0:4, 0:GW], [Hst.iv()], [("gscr", var, var + 1)], lane="gs%d" % var)
        for h in range(4):
            hank = bass.AP(tensor=gscr.tensor, offset=gscr[var * 4 + h, 0:1].offset, ap=[[1, 128], [1, HW]])
            dma("pool", Hdst.ap[:, h, :], hank, [("gscr", var, var + 1)], [Hdst.ivi(h)], lane="hk%d%d" % (var, h))

    if stop == "A":
        return finish([("kT", kT), ("qT", qT), ("Vaug", Vaug), ("utok", utok), ("mqT", mqT), ("stat0", stat0)])
    dma("pool", wmem.ap, w_mem.rearrange("(k p) n -> p k n", p=128), [], [wmem.iv()], lane="wmem")
    dma("sp", memf.ap, mem.rearrange("(t p) d -> p t d", p=128), [], [memf.iv()], lane="memf")
    memset("dve", wbd.ap, 0.0, [wbd.iv()])
    for g in range(4):
        j, r = g // 2, g % 2
        dma("sp", wbd.ap[64 * r:64 * r + 64, j, 64 * r:64 * r + 64], w_four[g], [], [wbd.iv()], lane="wbd", group=True)
    c64s = carve([128, 128], F32, at=R1 + 26624)
    s64s = carve([128, 128], F32, at=R1 + 27136)
    dma("sp", c64s.ap, c64_d, [], [c64s.iv()], lane="c64s", group=True)
    dma("sp", s64s.ap, s64_d, [], [s64s.iv()], lane="c64s", group=True)
    for j in range(2):
        for q_, cs_ in enumerate((c64s, s64s)):
            b = next_bank()
            mm(ps[:, b, 0:128], cs_.ap, wbd.ap[:, j, :], True, True, [cs_.iv(), wbd.iv()], [PSI(b, 0, 128)])
            evac(ABD.ap[:, j, q_ * 128:(q_ + 1) * 128], ps[:, b, 0:128], [PSI(b, 0, 128)], [ABD.iv(j * 256 + q_ * 128, j * 256 + q_ * 128 + 128)])

    def mem_pe():
        cp("pool", membf.ap, memf.ap, [memf.iv()], [membf.iv()])
        for tm in range(2):
            for k in range(8):
                tr(psb(tm)[:, k * 128:(k + 1) * 128], membf.ap[:, tm, k * 128:(k + 1) * 128], identb.ap,
                   [membf.iv(), identb.iv()], [PSI(tm)])
            evac(memT.ap[:, :, tm * 128:(tm + 1) * 128], psb(tm).rearrange("p (a b) -> p a b", a=8), [PSI(tm)], [memT.iv()])
        for j in range(2):
            b = next_bank()
            for k in range(8):
                mm(ps[:, b, 0:256], wmem.ap[:, k, 128 * j:128 * j + 128], memT.ap[:, k, :], k == 0, k == 7,
                   [wmem.iv(), memT.iv()], [PSI(b, 0, 256)])
            evac(mkT.ap[:, j, :], ps[:, b, 0:256], [PSI(b, 0, 256)], [mkT.ivi(j)])
        for tm in range(2):
            b = next_bank()
            for k in range(8):
                mm(ps[:, b, 0:256], memT.ap[:, k, tm * 128:(tm + 1) * 128], wmem.ap[:, k, 256:512], k == 0, k == 7,
                   [wmem.iv(), memT.iv()], [PSI(b, 0, 256)])
            evac(mvaug.ap[:, tm, :, 0:64], ps[:, b, 0:256].rearrange("p (a b) -> p a b", a=4), [PSI(b, 0, 256)], [mvaug.ivi(tm)])

    slot_i = [0]
    for sg in range(4):
        ybanks = {}
        for cs in range(2):
            pair = ((sg * 2 + cs) % 3) * 2
            for j in range(2):
                ybanks[(cs, j)] = pair + j
            for tq in range(4):
                sl = slot_i[0] % 3
                slot_i[0] += 1
                rg = ring[sl]
                dma("sp", rg.ap, dft[cs, sg, tq * 1024:(tq + 1) * 1024, :].rearrange("(t p) n -> p t n", p=128),
                    [], [rg.iv()], lane="ring%d" % sl)
                for tc in range(8):
                    t = tq * 8 + tc
                    for j in range(2):
                        b = pair + j
                        mm(ps[:, b, :], utok.ap[:, t, 128 * j:128 * j + 128], rg.ap[:, tc, :], t == 0, t == 31,
                           [utok.ivi(t), rg.ivi(tc)], [PSI(b)])
            for j in range(2):
                b = pair + j
                evac(ybuf.ap[:, cs * 2 + j, :], ps[:, b, :], [PSI(b)], [ybuf.ivi(cs * 2 + j)])
        for j in range(2):
            b = 6 + j
            mm(ps[:, b, :], ABD.ap[:, j, 0:128], ybuf.ap[:, j, :], True, False, [ABD.iv(), ybuf.ivi(j)], [PSI(b)])
            mm(ps[:, b, :], ABD.ap[:, j, 128:256], ybuf.ap[:, 2 + j, :], False, True, [ABD.iv(), ybuf.ivi(2 + j)], [PSI(b)])
            evac(oT.ap[:, 4 + j, sg * 512:(sg + 1) * 512], ps[:, b, :], [PSI(b)],
                 [oT.iv((4 + j) * OWN + sg * 512, (4 + j) * OWN + sg * 512 + 512)])
        if sg == 1:
            mem_pe()

    if stop == "four":
        return finish([("oT", oT)])
    selb2 = carve([128, 256], F32, at=ohs.off)
    cb2 = carve([128, 4, 256], F32, at=Hst.off)
    dma("sp", selb2.ap, selb2_d.partition_broadcast(128), [], [selb2.iv()], lane="selb2")
    for h in range(4):
        ts("dve", cb2.ap[:, h, :], selb2.ap, sm[:, 8 + h:9 + h], None, ALU.mult, None, [selb2.iv(), small.iv(8 + h, 9 + h)], [cb2.ivi(h)])
    zt_ = carve([128, D + 4], BF16, at=R1)
    memset("dve", zt_.ap, 0.0, [zt_.iv()])
    for tz in range(64):
        dma("sp", xs_z[tz * 128:(tz + 1) * 128, :], zt_.ap, [zt_.iv()], [("xs", 0, 64)], lane="xsz", group=True)
    wb_d = nc.dram_tensor("wb_scr", [NE * 128 * 3, 2048], BF16, kind="Internal").ap()
    wstg = [carve([128, 2048], BF16, at=R1 + 24576 + i * 4096) for i in range(2)]
    wp_v = wp_d.rearrange("(e p j) n -> e j p n", e=NE, p=128, j=3)
    wb_v = wb_d.rearrange("(e p j) n -> e j p n", e=NE, p=128, j=3)
    ci = 0
    for e_ in range(NE):
        for j in range(3):
            sg_ = wstg[ci % 2]
            dma("pool", sg_.ap, wp_v[e_, j], [], [sg_.iv()], lane="wcv%d" % (ci % 2))
            dma("sp", wb_v[e_, j], sg_.ap, [sg_.iv()], [("wb", 2 + ci, 3 + ci)], lane="wcw%d" % (ci % 2))
            ci += 1
    otok = carve([128, 16, 512], BF16, at=utok.off)
    otokm = carve([128, 16, 256], BF16, at=R1)
    SC = [[0, 2], [1, 3]]
    Qbd = [carve([128, 2, 256], BF16, at=R1 + 14336 + i * 1024) for i in range(2)]
    for i in range(2):
        memset("dve", Qbd[i].ap, 0.0, [Qbd[i].iv()])
    PTs = [[carve([128, 512], BF16, at=R1 + 8192 + (st * 3 + i) * 1024) for i in range(3)] for st in range(2)]

    def band2(g):
        return [(kb % 32) for kb in range(2 * g - 1, 2 * g + 3)]

    def stream_ops(h, g, st):
        q0 = g * 256
        band = band2(g)
        qb_ = Qbd[st]
        sbank = [2 * st, 2 * st + 1]
        abank = [4 + 2 * st, 5 + 2 * st]
        pts = PTs[st]

        def build():
            for c in range(2):
                cp("dve", qb_.ap[64 * c:64 * c + 64, c, :], qT.ap[64 * c:64 * c + 64, h, q0:q0 + 256],
                   [qT.iv(h * OWN + q0, h * OWN + q0 + 256)], [qb_.iv()])

        def qk(kb):
            b = sbank[kb % 2]
            isb = kb in band
            mm(ps[:, b, :], kT.ap[:, h, kb * 128:(kb + 1) * 128], qb_.ap.rearrange("p c q -> p (c q)"), True, not isb,
               [kT.iv(h * S + kb * 128, h * S + kb * 128 + 128), qb_.iv()], [PSI(b)])
            if isb:
                wrap = (g == 0 and kb == 31) or (g == 7 and kb == 16)
                a_ = 256 * g - 128 * kb
                if wrap:
                    a_ = 128 if g == 0 else -256
                j0 = a_ + 512
                Hs = H2b if wrap else Hb
                for c in range(2):
                    mm(ps[:, b, c * 256:(c + 1) * 256], jb.ap, Hs.ap[:, h, j0:j0 + 256], False, c == 1, [jb.iv(), Hs.ivi(h)], [PSI(b)])

        def ex(kb):
            b = sbank[kb % 2]
            pt = pts[kb % 3]
            if (kb in band) or (kb < 16 and kb > 2 * g + 2):
                act(pt.ap, ps[:, b, :], AF.Exp, [PSI(b)], [pt.iv()])
            else:
                pi = g * 32 + kb
                act(pt.ap, ps[:, b, :], AF.Exp, [PSI(b), cb2.ivi(h)], [pt.iv()], bias=cb2.ap[:, h, pi:pi + 1])

        def av(kb):
            pt = pts[kb % 3]
            for qb in range(2):
                for c in range(2):
                    mm(ps[:, abank[qb], c * 130:c * 130 + 129], pt.ap[:, c * 256 + qb * 128:c * 256 + (qb + 1) * 128],
                       Vaug.ap[:, kb, h, 0:129], (kb == 0 and c == 0), (kb == 31),
                       [pt.iv(), Vaug.ivi(kb)], [PSI(abank[qb], c * 130, c * 130 + 129)], skip=True)

        def copies():
            for qb in range(2):
                a_s = accS4[st][qb]
                cp("dve", a_s.ap[:, 0:260], ps[:, abank[qb], 0:260], [PSI(abank[qb], 0, 260)], [a_s.iv()])

        def part1():
            for qb in range(2):
                j = st * 2 + qb
                a_s = accS4[st][qb]
                A = a_s.ap
                pm = psm.ap
                P.add("dve", lambda e, A=A, pm=pm, j=j: e.reciprocal(out=pm[:, j:j + 1], in_=A[:, 128:129]), reads=[a_s.iv()], writes=[psm.iv(j, j + 1)])
                P.add("dve", lambda e, A=A, pm=pm, j=j: e.reciprocal(out=pm[:, 4 + j:5 + j], in_=A[:, 258:259]), reads=[a_s.iv()], writes=[psm.iv(4 + j, 5 + j)])
                tt("dve", pm[:, 4 + j:5 + j], pm[:, 4 + j:5 + j], sm[:, 4:5], ALU.mult, [psm.iv(4 + j, 5 + j), small.iv(4, 5)], [psm.iv(4 + j, 5 + j)])
                ts("dve", A[:, 0:128], A[:, 0:128], pm[:, j:j + 1], None, ALU.mult, None, [a_s.iv(), psm.iv(j, j + 1)], [a_s.iv()])
                stt_(A[:, 0:128], A[:, 130:258], pm[:, 4 + j:5 + j], A[:, 0:128], ALU.mult, ALU.add, [a_s.iv(), psm.iv(4 + j, 5 + j)], [a_s.iv()])
                stt_(A[:, 130:258], A[:, 0:128], 1.0, A[:, 0:128], ALU.mult, ALU.mult, [a_s.iv()], [a_s.iv(), psm.iv(8 + j, 9 + j)],
                     accum=pm[:, 8 + j:9 + j])

        def part3():
            for qb in range(2):
                j = st * 2 + qb
                tile = g * 2 + qb
                a_s = accS4[st][qb]
                stt_(otok.ap[:, tile, h * 128:(h + 1) * 128], a_s.ap[:, 0:128], psm.ap[:, 12 + j:13 + j], gsub.ap, ALU.mult, ALU.mult,
                     [a_s.iv(), psm.iv(12 + j, 13 + j), gsub.iv()], [otok.iv(tile * 512 + h * 128, tile * 512 + h * 128 + 128)])

        return dict(build=build, qk=qk, ex=ex, av=av, copies=copies, part1=part1, part3=part3)

    accS4 = [[carve([128, 264], F32, at=R1 + 16384 + (st_ * 2 + qb_) * 1088) for qb_ in range(2)] for st_ in range(2)]
    psm = carve([128, 16], F32, at=R1 + 20736)

    def part2():
        act(psm.ap[:, 12:16], psm.ap[:, 8:12], AF.Ln, [psm.iv(8, 12)], [psm.iv(12, 16)], bias=EPS, scale=1.0 / 128.0)
        act(psm.ap[:, 12:16], psm.ap[:, 12:16], AF.Exp, [psm.iv(12, 16)], [psm.iv(12, 16)], scale=-0.5)

    pending = None
    for h in range(4):
        for i in range(4):
            A_ = stream_ops(h, 2 * i, 0)
            B_ = stream_ops(h, 2 * i + 1, 1)
            A_["build"]()
            B_["build"]()
            for kb0 in range(2):
                A_["qk"](kb0)
                B_["qk"](kb0)
            if pending is not None:
                pending[0]["part1"]()
                pending[1]["part1"]()
            for kb in range(32):
                for S_ in (A_, B_):
                    S_["ex"](kb)
                    S_["av"](kb)
                    if kb + 2 < 32:
                        S_["qk"](kb + 2)
                if pending is not None and kb == 12:
                    part2()
                if pending is not None and kb == 20:
                    pending[0]["part3"]()
                    pending[1]["part3"]()
            A_["copies"]()
            B_["copies"]()
            pending = (A_, B_)
    pending[0]["part1"]()
    pending[1]["part1"]()
    part2()
    pending[0]["part3"]()
    pending[1]["part3"]()

    accM = [accS4[0][0], accS4[0][1], accS4[1][0], accS4[1][1]]
    msteps = [(qg, mh, j) for qg in range(4) for mh in range(4) for j in range(2)]

    def m_qk(si):
        qg, mh, j = msteps[si]
        jj, r = mh // 2, mh % 2
        q0 = qg * 512
        b = si % 3
        mm(ps[:, b, :], mkT.ap[64 * r:64 * r + 64, jj, j * 128:(j + 1) * 128], mqT.ap[64 * r:64 * r + 64, jj, q0:q0 + 512],
           True, True, [mkT.ivi(jj), mqT.iv(jj * OWN + q0, jj * OWN + q0 + 512)], [PSI(b)])

    def m_ex(si):
        b = si % 3
        pt = PT[0][si % 3]
        act(pt.ap, ps[:, b, :], AF.Exp, [PSI(b)], [pt.iv()])

    def m_av(si):
        qg, mh, j = msteps[si]
        pt = PT[0][si % 3]
        for qb in range(4):
            mm(ps[:, 4 + qb, mh * 66:mh * 66 + 65], pt.ap[:, qb * 128:(qb + 1) * 128], mvaug.ap[:, j, mh, 0:65],
               (mh == 0 and j == 0), (j == 1), [pt.iv(), mvaug.ivi(j)], [PSI(4 + qb, mh * 66, mh * 66 + 65)], skip=True)

    def m_post_copy(qg):
        for qb in range(4):
            cp("dve", accM[qb].ap[:, 0:264], ps[:, 4 + qb, 0:264], [PSI(4 + qb, 0, 264)], [accM[qb].iv()])

    def m_post_math(qg):
        for qb in range(4):
            tile = qg * 4 + qb
            a_ = accM[qb]
            for mh in range(4):
                P.add("dve", lambda e, A=a_.ap, mh=mh: e.reciprocal(out=A[:, mh * 66 + 64:mh * 66 + 65], in_=A[:, mh * 66 + 64:mh * 66 + 65]),
                      reads=[a_.iv()], writes=[a_.iv()])
                ts("dve", otokm.ap[:, tile, mh * 64:(mh + 1) * 64], a_.ap[:, mh * 66:mh * 66 + 64], a_.ap[:, mh * 66 + 64:mh * 66 + 65], None,
                   ALU.mult, None, [a_.iv()], [otokm.ivi(tile)])

    def d_tr(t):
        for cch in range(4):
            tr(psb(3)[:, cch * 128:(cch + 1) * 128], otok.ap[:, t, cch * 128:(cch + 1) * 128], identb.ap,
               [otok.ivi(t), identb.iv()], [PSI(3)])
        evac(oT.ap[:, 0:4, t * 128:(t + 1) * 128], psb(3)[:, 0:512].rearrange("p (a b) -> p a b", a=4),
             [PSI(3)], [oT.iv(0, 4 * OWN)])

    m_qk(0)
    m_qk(1)
    pend = None
    for si in range(32):
        qg, mh, j = msteps[si]
        m_ex(si)
        m_av(si)
        if si + 2 < 32:
            m_qk(si + 2)
        if si % 2 == 0:
            d_tr(si // 2)
        if mh == 3 and j == 1:
            m_post_copy(qg)
            pend = qg
        if pend is not None and (si % 8) == 3:
            m_post_math(pend)
            pend = None
    m_post_math(3)

    if stop == "attn":
        return finish([("otok", otok), ("otokm", otokm)])
    for t in range(16):
        tb = t % 2
        for cch in range(2):
            tr(psb(tb)[:, cch * 128:(cch + 1) * 128], otokm.ap[:, t, cch * 128:(cch + 1) * 128], identb.ap,
               [otokm.ivi(t), identb.iv()], [PSI(tb)])
        evac(oT.ap[:, 6:8, t * 128:(t + 1) * 128], psb(tb)[:, 0:256].rearrange("p (a b) -> p a b", a=2),
             [PSI(tb)], [oT.iv(6 * OWN, 8 * OWN)])

    if stop == "oT":
        return finish([("oT", oT)])
    base = 0
    oh_free = base + 100352
    acc = carve([128, 16, D], F32, at=base)
    h1T = carve([128, 8, OWN], BF16, at=base + 65536)
    gates = carve([128, 16, 32], F32, at=base + 98304)
    LG = carve([128, 16, 36], F32, at=oh_free)
    gw = carve([128, 16, 64], F32, at=R1 + 16384)
    em = carve([128, 16, 32], F32, at=R1 + 20480)
    gw2 = carve([128, 16, 64], F32, at=R1 + 24576)
    woutb = carve([128, 8, D], BF16, at=qT.off)
    gb = carve([128, 2, D], F32, at=Hb.off + 8192)
    gb1 = carve([128, 2, D], F32, at=Hb.off)
    xr = [carve([128, D], F32, at=R1 + 24576 + i * 4096) for i in range(2)]
    zt = [carve([128, D], F32, at=Hst.off + i * 4096) for i in range(1)]
    zt = [carve([128, D], F32, at=ohs.off)]
    h1Tf = carve([128, 8, 128], F32, at=R1)
    wrs = carve([128, 8, 36], F32, at=Hb.off + 16384)
    bcat = carve([128, 36], F32, at=Hb.off + 16384 + 1152)
    assert Hb.off + 16384 + 1152 + 144 <= Hst.off

    dma("pool", woutb.ap, w_out.rearrange("(k p) n -> p k n", p=128), [], [woutb.iv()], lane="wout")
    dma("sp", gb.ap[:, 0, :], ln0_g.partition_broadcast(128), [], [gb.ivi(0)], lane="gbC", group=True)
    dma("sp", gb.ap[:, 1, :], ln0_b.partition_broadcast(128), [], [gb.ivi(1)], lane="gbC", group=True)
    dma("sp", gb1.ap[:, 0, :], ln1_g.partition_broadcast(128), [], [gb1.ivi(0)], lane="gbC", group=True)
    dma("sp", gb1.ap[:, 1, :], ln1_b.partition_broadcast(128), [], [gb1.ivi(1)], lane="gbC", group=True)
    dma("sp", wrs.ap, wr_cat.rearrange("(k p) n -> p k n", p=128), [], [wrs.iv()], lane="gbC", group=True)
    dma("sp", bcat.ap, b_cat.partition_broadcast(128), [], [bcat.iv()], lane="gbC", group=True)
    act(gb.ap, gb.ap, AF.Copy, [gb.iv()], [gb.iv()], scale=ALPHA)
    act(gb1.ap, gb1.ap, AF.Copy, [gb1.iv()], [gb1.iv()], scale=ALPHA)

    xr = xr + [carve([128, D], F32, at=ohs.off)]
    h1Tfs = [h1Tf, carve([128, 8, 128], F32, at=Hst.off)]
    stat1 = carve([128, 16, 16], F32, at=base + 102656)

    def s1_mm(t):
        b0 = 2 * (t % 2)
        for hf in range(2):
            for k in range(8):
                mm(ps[:, b0 + hf, :], oT.ap[:, k, t * 128:(t + 1) * 128], woutb.ap[:, k, hf * 512:(hf + 1) * 512], k == 0, k == 7,
                   [oT.iv(k * OWN + t * 128, k * OWN + t * 128 + 128), woutb.ivi(k)], [PSI(b0 + hf)])

    def s1_front(t):
        xr_ = xr[t % 3]
        dma("sp", xr_.ap, x[t * 128:(t + 1) * 128, :], [], [xr_.iv()], lane="xr%d" % (t % 3))
        act(xr_.ap, xr_.ap, AF.Identity, [xr_.iv(), stat0.iv(2 * t, 2 * t + 2)], [xr_.iv()],
            bias=stat0.ap[:, t, 1:2], scale=stat0.ap[:, t, 0:1])
        tt("dve", xr_.ap, xr_.ap, gb.ap[:, 0, :], ALU.mult, [xr_.iv(), gb.ivi(0)], [xr_.iv()])

    def s1_mid(t):
        xr_ = xr[t % 3]
        tt("pool", xr_.ap, xr_.ap, gb.ap[:, 1, :], ALU.add, [xr_.iv(), gb.ivi(1)], [xr_.iv()])

    def s1_back(t):
        xr_ = xr[t % 3]
        b0 = 2 * (t % 2)
        a_ap = acc.ap[:, t, :]
        tt("dve", a_ap, ps[:, b0:b0 + 2, :].rearrange("p a b -> p (a b)"), xr_.ap, ALU.add, [PSI(b0, 0, 512, nb=2), xr_.iv()], [acc.ivi(t)])
        s1 = stat1.ap[:, t, :]
        P.add("dve", lambda e: e.bn_stats(out=s1[:, 0:6], in_=a_ap[:, 0:512]), reads=[acc.ivi(t)], writes=[stat1.ivi(t)])
        P.add("dve", lambda e: e.bn_stats(out=s1[:, 6:12], in_=a_ap[:, 512:1024]), reads=[acc.ivi(t)], writes=[stat1.ivi(t)])
        P.add("dve", lambda e: e.bn_aggr(out=s1[:, 12:14], in_=s1[:, 0:12]), reads=[stat1.ivi(t)], writes=[stat1.ivi(t)])

    for i in range(18):
        if 0 <= i - 2 < 16:
            s1_back(i - 2)
        if 0 <= i - 1 < 16:
            s1_mid(i - 1)
        if i < 16:
            s1_mm(i)
            s1_front(i)
    act(stat1.ap[:, :, 14], stat1.ap[:, :, 13], AF.Ln, [stat1.iv()], [stat1.iv()], bias=EPS, scale=1.0)
    act(stat1.ap[:, :, 14], stat1.ap[:, :, 14], AF.Exp, [stat1.iv()], [stat1.iv()], scale=-0.5)
    stt_(stat1.ap[:, :, 15], stat1.ap[:, :, 12], -1.0, stat1.ap[:, :, 14], ALU.mult, ALU.mult, [stat1.iv()], [stat1.iv()])
    def s5_T(t):
        bq = 4 + 2 * (t % 2)
        hf_ = h1Tfs[t % 2]
        for k in range(8):
            tr(ps[:, bq + k // 4, (k % 4) * 128:(k % 4 + 1) * 128], acc.ap[:, t, k * 128:(k + 1) * 128], identf.ap,
               [acc.ivi(t), identf.iv()], [PSI(bq, 0, 512, nb=2)])
        for hh in range(2):
            act(hf_.ap[:, 4 * hh:4 * hh + 4, :], ps[:, bq + hh, :].rearrange("p (a b) -> p a b", a=4), AF.Copy,
                [PSI(bq, 0, 512, nb=2)], [hf_.iv(hh * 512, hh * 512 + 512)], scale=1.0 / ALPHA)

    def s5_R(t):
        hf_ = h1Tfs[t % 2]
        bl = t % 2
        for k in range(8):
            mm(ps[:, bl, 0:36], hf_.ap[:, k, :], wrs.ap[:, k, :], k == 0, k == 7, [hf_.ivi(k), wrs.ivi(k)], [PSI(bl, 0, 36)])
        tt("dve", LG.ap[:, t, :], ps[:, bl, 0:36], bcat.ap, ALU.add, [PSI(bl, 0, 36), bcat.iv()], [LG.ivi(t)])

    def s4(t):
        a_ap = acc.ap[:, t, :]
        act(a_ap, a_ap, AF.Identity, [acc.ivi(t), stat1.iv()], [acc.ivi(t)], bias=stat1.ap[:, t, 15:16], scale=stat1.ap[:, t, 14:15])
        tt("dve", a_ap, a_ap, gb1.ap[:, 0, :], ALU.mult, [acc.ivi(t), gb1.ivi(0)], [acc.ivi(t)])
        tt("pool", a_ap, a_ap, gb1.ap[:, 1, :], ALU.add, [acc.ivi(t), gb1.ivi(1)], [acc.ivi(t)])

    for i in range(16 + 3):
        if i < 16:
            s4(i)
        if 0 <= i - 2 < 16:
            s5_T(i - 2)
        if 0 <= i - 3 < 16:
            s5_R(i - 3)

    W = gw.ap
    wiv = gw.iv()
    GLa = LG.ap[:, :, 0:4]
    ELa = LG.ap[:, :, 4:36]
    gmax = W[:, :, 0:1]
    P.add("dve", lambda e: e.tensor_reduce(out=W[:, :, 0], in_=GLa, axis=AX.X, op=ALU.max), reads=[LG.iv()], writes=[wiv])
    tt("dve", W[:, :, 4:8], GLa, gmax.broadcast_to([128, 16, 4]), ALU.is_ge, [LG.iv(), wiv], [wiv])
    tt("dve", W[:, :, 8:12], GLa, gmax.broadcast_to([128, 16, 4]), ALU.subtract, [LG.iv(), wiv], [wiv])
    act(W[:, :, 8:12], W[:, :, 8:12], AF.Exp, [wiv], [wiv])
    P.add("dve", lambda e: e.tensor_reduce(out=W[:, :, 1], in_=W[:, :, 8:12], axis=AX.X, op=ALU.add), reads=[wiv], writes=[wiv])
    P.add("dve", lambda e: e.reciprocal(out=W[:, :, 2], in_=W[:, :, 1]), reads=[wiv], writes=[wiv])
    ts("dve", W[:, :, 12:16], W[:, :, 4:8], -1.0, 1e30, ALU.add, ALU.mult, [wiv], [wiv])
    EM = em.ap
    tt("dve", EM.rearrange("p t (g e) -> p t g e", g=4), ELa.rearrange("p t (g e) -> p t g e", g=4),
       W[:, :, 12:16].unsqueeze(3).broadcast_to([128, 16, 4, 8]), ALU.add, [LG.iv(), wiv], [em.iv()])
    for t in range(16):
        P.add("dve", lambda e, t=t: e.max(out=W[:, t, 16:24], in_=EM[:, t, :]), reads=[em.ivi(t)], writes=[wiv])
    tt("dve", W[:, :, 24], W[:, :, 17], W[:, :, 16], ALU.subtract, [wiv], [wiv])
    act(W[:, :, 25], W[:, :, 24], AF.Exp, [wiv], [wiv])
    ts("dve", W[:, :, 26], W[:, :, 25], 1.0, None, ALU.add, None, [wiv], [wiv])
    P.add("dve", lambda e: e.reciprocal(out=W[:, :, 27], in_=W[:, :, 26]), reads=[wiv], writes=[wiv])
    tt("dve", W[:, :, 27], W[:, :, 27], W[:, :, 2], ALU.mult, [wiv], [wiv])
    tt("dve", W[:, :, 28], W[:, :, 27], W[:, :, 25], ALU.mult, [wiv], [wiv])
    D0 = Hb.off
    gsel = carve([128, 16, 2], F32, at=D0)
    slots_f = carve([128, 32], F32, at=D0 + 128)
    slots_i = carve([128, 32], I32, at=D0 + 256)
    widx_i = carve([128, 192], I32, at=D0 + 512)
    widx_f = carve([128, 192], F32, at=D0 + 77824)
    maskb = carve([128, 16, 32], F32, at=D0 + 1536)
    oh1 = carve([128, 16, 32], F32, at=D0 + 1536 + 2048)
    oh2 = carve([128, 16, 32], F32, at=D0 + 1536 + 4096)
    within = carve([128, 16, 32], F32, at=D0 + 1536 + 6144)
    cntb = carve([128, 16, 32], F32, at=D0 + 1536 + 8192)
    baseb = carve([128, 16, 32], F32, at=D0 + 1536 + 10240)
    slotm = carve([128, 16, 32], F32, at=D0 + 1536 + 12288)
    thr = carve([128, 32, 16], F32, at=D0 + 1536 + 14336)
    tcmp = carve([128, 64, 32], F32, at=D0 + 17920)
    smallr = carve([128, 512], F32, at=D0 + 26112)
    Uc = carve([128, 128], F32, at=D0 + 28160)
    onesc = carve([128, 128], F32, at=D0 + 28672)
    tauc = carve([128, 64], F32, at=D0 + 29184)
    pjc = carve([128, 4], F32, at=D0 + 29440)
    tt("dve", oh1.ap, EM, W[:, :, 16:17].broadcast_to([128, 16, 32]), ALU.is_equal, [em.iv(), wiv], [oh1.iv()])
    tt("dve", oh2.ap, EM, W[:, :, 17:18].broadcast_to([128, 16, 32]), ALU.is_equal, [em.iv(), wiv], [oh2.iv()])
    cp("dve", gsel.ap, W[:, :, 27:29], [wiv], [gsel.iv()])

    if stop == "C":
        return finish([("acc", acc), ("oh1", oh1), ("gsel", gsel)])
    NT = 64
    xs_d = xs_z
    ys_d = nc.dram_tensor("ys_scr", [NT * 128, D], BF16, kind="Internal").ap()
    dma("sp", Uc.ap, u_d, [], [Uc.iv()], lane="rc", group=True)
    dma("sp", onesc.ap, ones_d, [], [onesc.iv()], lane="rc", group=True)
    dma("sp", tauc.ap, tau_d.partition_broadcast(128), [], [tauc.iv()], lane="rc", group=True)
    dma("sp", pjc.ap, pj_d, [], [pjc.iv()], lane="rc", group=True)
    dma("sp", thr.ap, thr_d.partition_broadcast(128), [], [thr.iv()], lane="rc", group=True)
    gb2 = carve([128, 2, D], F32, at=D0 + 61440)
    yo = [carve([128, D], F32, at=D0 + 69632 + i * 4096) for i in range(2)]
    tt("dve", maskb.ap, oh1.ap, oh2.ap, ALU.add, [oh1.iv(), oh2.iv()], [maskb.iv()])
    mflat = maskb.ap.rearrange("p t e -> p (t e)")
    mm(ps[:, 0, :], Uc.ap, mflat, True, True, [Uc.iv(), maskb.iv()], [PSI(0)])
    mm(ps[:, 1, :], onesc.ap, mflat, True, True, [onesc.iv(), maskb.iv()], [PSI(1)])
    cp("dve", within.ap.rearrange("p t e -> p (t e)"), ps[:, 0, :], [PSI(0)], [within.iv()])
    cp("dve", cntb.ap.rearrange("p t e -> p (t e)"), ps[:, 1, :], [PSI(1)], [cntb.iv()])
    memset("dve", baseb.ap[:, 0, :], 0.0, [baseb.ivi(0)])
    for t in range(1, 16):
        tt("dve", baseb.ap[:, t, :], baseb.ap[:, t - 1, :], cntb.ap[:, t - 1, :], ALU.add, [baseb.ivi(t - 1), cntb.ivi(t - 1)], [baseb.ivi(t)])
    R_ = smallr.ap
    riv = smallr.iv()
    tt("dve", R_[:, 0:32], baseb.ap[:, 15, :], cntb.ap[:, 15, :], ALU.add, [baseb.ivi(15), cntb.ivi(15)], [riv])
    tt("dve", tcmp.ap.rearrange("p a b -> p (a b)")[:, 0:512].rearrange("p (e j) -> p e j", e=32),
       R_[:, 0:32].unsqueeze(2).broadcast_to([128, 32, 16]), thr.ap, ALU.is_gt, [riv, thr.iv()], [tcmp.iv()])
    P.add("dve", lambda e: e.tensor_reduce(out=R_[:, 32:64], in_=tcmp.ap.rearrange("p a b -> p (a b)")[:, 0:512].rearrange("p (e j) -> p e j", e=32),
                                           axis=AX.X, op=ALU.add), reads=[tcmp.iv()], writes=[riv])
    memset("dve", R_[:, 64:65], 0.0, [riv])
    for e_ in range(1, 32):
        tt("dve", R_[:, 64 + e_:65 + e_], R_[:, 63 + e_:64 + e_], R_[:, 31 + e_:32 + e_], ALU.add, [riv], [riv])
    ts("dve", R_[:, 96:128], R_[:, 64:96], 128.0, None, ALU.mult, None, [riv], [riv])
    tt("dve", slotm.ap, baseb.ap, within.ap, ALU.add, [baseb.iv(), within.iv()], [slotm.iv()])
    tt("dve", slotm.ap, slotm.ap, R_[:, 96:128].unsqueeze(1).broadcast_to([128, 16, 32]), ALU.add, [slotm.iv(), riv], [slotm.iv()])
    tt("dve", oh1.ap, oh1.ap, slotm.ap, ALU.mult, [oh1.iv(), slotm.iv()], [oh1.iv()])
    tt("dve", oh2.ap, oh2.ap, slotm.ap, ALU.mult, [oh2.iv(), slotm.iv()], [oh2.iv()])
    P.add("dve", lambda e: e.tensor_reduce(out=slots_f.ap[:, 0:16], in_=oh1.ap, axis=AX.X, op=ALU.add), reads=[oh1.iv()], writes=[slots_f.iv()])
    P.add("dve", lambda e: e.tensor_reduce(out=slots_f.ap[:, 16:32], in_=oh2.ap, axis=AX.X, op=ALU.add), reads=[oh2.iv()], writes=[slots_f.iv()])
    cp("dve", slots_i.ap, slots_f.ap, [slots_f.iv()], [slots_i.iv()])
    tt("dve", tcmp.ap, R_[:, 64:96].unsqueeze(1).broadcast_to([128, 64, 32]), tauc.ap.unsqueeze(2).broadcast_to([128, 64, 32]),
       ALU.is_le, [riv, tauc.iv()], [tcmp.iv()])
    P.add("dve", lambda e: e.tensor_reduce(out=R_[:, 128:192], in_=tcmp.ap, axis=AX.X, op=ALU.add), reads=[tcmp.iv()], writes=[riv])
    ts("dve", R_[:, 128:192], R_[:, 128:192], -1.0, 384.0, ALU.add, ALU.mult, [riv], [riv])
    tt("dve", widx_f.ap.rearrange("p (a j) -> p a j", j=3), R_[:, 128:192].unsqueeze(2).broadcast_to([128, 64, 3]),
       pjc.ap[:, 0:3].unsqueeze(1).broadcast_to([128, 64, 3]), ALU.add, [riv, pjc.iv()], [widx_f.iv()])
    ts("dve", widx_f.ap[:, 0:64], R_[:, 128:192], 1.0 / 3.0, pjc.ap[:, 3:4], ALU.mult, ALU.add, [riv, pjc.iv()], [widx_f.iv()])
    cp("dve", widx_i.ap, widx_f.ap, [widx_f.iv()], [widx_i.iv()])
    if stop == "R":
        return finish([("slots_f", slots_f), ("smallr", smallr), ("widx_f", widx_f)])
    h1tok = [carve([128, D + 4], BF16, at=o_) for o_ in (98304, 100416, 103680, D0 + 29696, 90112, 92224, 94336, D0 + 57344)] + \
            [carve([128, D + 4], BF16, at=D0 + 45056 + i * 2112) for i in range(5)] + \
            [carve([128, D + 4], BF16, at=D0 + 78592 + i * 2112) for i in range(3)]
    gpc = carve([128, 32, 4], BF16, at=D0 + 26112 + 1536)
    gtmp = carve([128, 32], F32, at=D0 + 26112 + 1536 + 256)
    gflat = gsel.ap.rearrange("p t k -> p (t k)")
    memset("dve", gpc.ap, 0.0, [gpc.iv()])
    cp("dve", gpc.ap[:, :, 0], gflat, [gsel.iv()], [gpc.iv()])
    tt("dve", gtmp.ap, gflat, gpc.ap[:, :, 0], ALU.subtract, [gsel.iv(), gpc.iv()], [gtmp.iv()])
    cp("dve", gpc.ap[:, :, 1], gtmp.ap, [gtmp.iv()], [gpc.iv()])
    tt("dve", gtmp.ap, gtmp.ap, gpc.ap[:, :, 1], ALU.subtract, [gtmp.iv(), gpc.iv()], [gtmp.iv()])
    cp("dve", gpc.ap[:, :, 2], gtmp.ap, [gtmp.iv()], [gpc.iv()])
    XSW = ("xs", 0, 1)
    YSW = ("ys", 0, 1)
    for t in range(16):
        for k in range(2):
            hb_ = h1tok[(t % 8) * 2 + k]
            col = k * 16 + t
            act(hb_.ap[:, 0:D], acc.ap[:, t, :], AF.Copy, [acc.ivi(t)], [hb_.iv(0, D)], scale=1.0 / ALPHA)
            cp("dve", hb_.ap[:, D:D + 4], gpc.ap[:, t * 2 + k, :], [gpc.iv()], [hb_.iv(D, D + 4)])
            P.add("pool", lambda e, hb_=hb_, col=col: e.indirect_dma_start(
                out=xs_d, out_offset=bass.IndirectOffsetOnAxis(ap=slots_i.ap[:, col:col + 1].bitcast(U32), axis=0),
                in_=hb_.ap, in_offset=None), reads=[hb_.iv(), slots_i.iv()], writes=[("xs", 2 + col, 3 + col)], lane="sc%d" % ((t % 8) * 2 + k))
    wbuf = [carve([128, 6144], BF16, at=o_) for o_ in (65536, 77824, D0 + 45056, D0 + 78592)]
    NWB = len(wbuf)
    assert D0 + 78592 + 12288 <= SBUF_BYTES
    wb_rows = wb_d.rearrange("(r j) n -> r (j n)", j=3)
    xst = [carve([128, D + 4], BF16, at=90112), carve([128, D + 4], BF16, at=D0 + 57344)]
    gcol = smallr.ap[:, 320:324]
    XsT = [carve([128, 8, 128], BF16, at=94208 + i * 2048) for i in range(2)]
    sa = [carve([128, 256], BF16, at=D0 + 33792 + i * 512) for i in range(2)]
    hid = [carve([128, 256], BF16, at=D0 + 34816 + i * 512) for i in range(2)]
    hidT = [carve([128, 2, 128], BF16, at=D0 + 35840 + i * 512) for i in range(2)]
    ysb = [carve([128, D], BF16, at=D0 + 36864 + i * 4096) for i in range(2)]
    allxs = [("xs", 0, 64)]
    def a_T(tau):
        q = tau % 2
        wb_ = wbuf[tau % NWB]
        P.add("pool", lambda e, wb_=wb_, tau=tau: e.indirect_dma_start(
            out=wb_.ap, out_offset=None, in_=wb_rows,
            in_offset=bass.IndirectOffsetOnAxis(ap=widx_i.ap[:, tau:tau + 1].bitcast(U32), axis=0)),
            reads=[widx_i.iv(), ("wb", 0, 128)], writes=[wb_.iv()], lane="wg%d" % (tau % NWB))
        if tau == 0:
            for t2 in range(2):
                dma("sp", xst[t2].ap, xs_d[t2 * 128:(t2 + 1) * 128, :], allxs, [xst[t2].iv()], lane="xst%d" % t2)
        for k in range(8):
            tr(psb(0)[:, k * 128:(k + 1) * 128], xst[q].ap[:, k * 128:(k + 1) * 128], identb.ap, [xst[q].iv(), identb.iv()], [PSI(0)])
        P.add("dve", lambda e, q=q: e.tensor_reduce(out=gcol[:, q:q + 1], in_=xst[q].ap[:, D:D + 3], axis=AX.X, op=ALU.add),
              reads=[xst[q].iv()], writes=[smallr.iv(320 + q, 321 + q)])
        if tau + 2 < NT:
            dma("sp", xst[q].ap, xs_d[(tau + 2) * 128:(tau + 3) * 128, :], allxs, [xst[q].iv()], lane="xst%d" % q)
        evac(XsT[q].ap.rearrange("p a b -> p (a b)"), psb(0), [PSI(0)], [XsT[q].iv()])

    def a_up(tau):
        q = tau % 2
        wb_ = wbuf[tau % NWB]
        for k in range(8):
            mm(ps[:, 2 + q, :], XsT[q].ap[:, k, :], wb_.ap[:, k * 512:(k + 1) * 512], k == 0, k == 7,
               [XsT[q].ivi(k), wb_.iv(k * 512, (k + 1) * 512)], [PSI(2 + q)])
        act(sa[q].ap, ps[:, 2 + q, 0:256], AF.Silu, [PSI(2 + q)], [sa[q].iv()])
        tt("dve", hid[q].ap, sa[q].ap, ps[:, 2 + q, 256:512], ALU.mult, [sa[q].iv(), PSI(2 + q)], [hid[q].iv()])

    def b_(tau):
        q = tau % 2
        wb_ = wbuf[tau % NWB]
        for c in range(2):
            tr(psb(1)[:, c * 128:(c + 1) * 128], hid[q].ap[:, c * 128:(c + 1) * 128], identb.ap, [hid[q].iv(), identb.iv()], [PSI(1)])
        evac(hidT[q].ap.rearrange("p a b -> p (a b)"), psb(1)[:, 0:256], [PSI(1)], [hidT[q].iv()])
        for hf in range(2):
            for c in range(2):
                mm(ps[:, 4 + 2 * q + hf, :], hidT[q].ap[:, c, :], wb_.ap[:, 4096 + c * 1024 + hf * 512:4096 + c * 1024 + hf * 512 + 512], c == 0, c == 1,
                   [hidT[q].ivi(c), wb_.iv(4096, 6144)], [PSI(4 + 2 * q + hf)])
        act(ysb[q].ap[:, 0:512], ps[:, 4 + 2 * q, :], AF.Identity, [PSI(4 + 2 * q), smallr.iv(320 + q, 321 + q)], [ysb[q].iv(0, 512)], scale=gcol[:, q:q + 1])
        ts("dve", ysb[q].ap[:, 512:1024], ps[:, 5 + 2 * q, :], gcol[:, q:q + 1], None, ALU.mult, None, [PSI(5 + 2 * q), smallr.iv(320 + q, 321 + q)], [ysb[q].iv(512, 1024)])
        dma("sp", ys_d[tau * 128:(tau + 1) * 128, :], ysb[q].ap, [ysb[q].iv()], [("ys", 2 + tau, 3 + tau)], lane="ysw%d" % q)

    a_T(0)
    a_up(0)
    for tau in range(NT):
        if tau + 1 < NT:
            a_T(tau + 1)
        b_(tau)
        if tau + 1 < NT:
            a_up(tau + 1)
    dma("sp", gb2.ap[:, 0, :], ln2_g.partition_broadcast(128), [], [gb2.ivi(0)], lane="gbE", group=True)
    dma("sp", gb2.ap[:, 1, :], ln2_b.partition_broadcast(128), [], [gb2.ivi(1)], lane="gbE", group=True)
    if stop == "D":
        return finish([("acc", acc)])
    allys = [("ys", 0, 128)]
    for k in range(2):
        for t in range(16):
            a_ap = acc.ap[:, t, :]
            col = k * 16 + t
            P.add("pool", lambda e, a_ap=a_ap, col=col: e.indirect_dma_start(
                out=a_ap, out_offset=None, in_=ys_d,
                in_offset=bass.IndirectOffsetOnAxis(ap=slots_i.ap[:, col:col + 1].bitcast(U32), axis=0), compute_op=ALU.add),
                reads=[slots_i.iv(), acc.ivi(t)] + allys, writes=[acc.ivi(t)], lane="yg%d" % t)
    for t in range(16):
        a_ap = acc.ap[:, t, :]
        s1 = stat1.ap[:, t, :]
        P.add("dve", lambda e, s1=s1, a_ap=a_ap: e.bn_stats(out=s1[:, 0:6], in_=a_ap[:, 0:512]), reads=[acc.ivi(t)], writes=[stat1.ivi(t)])
        P.add("dve", lambda e, s1=s1, a_ap=a_ap: e.bn_stats(out=s1[:, 6:12], in_=a_ap[:, 512:1024]), reads=[acc.ivi(t)], writes=[stat1.ivi(t)])
        P.add("dve", lambda e, s1=s1: e.bn_aggr(out=s1[:, 12:14], in_=s1[:, 0:12]), reads=[stat1.ivi(t)], writes=[stat1.ivi(t)])
    act(stat1.ap[:, :, 14], stat1.ap[:, :, 13], AF.Ln, [stat1.iv()], [stat1.iv()], bias=EPS, scale=1.0)
    act(stat1.ap[:, :, 14], stat1.ap[:, :, 14], AF.Exp, [stat1.iv()], [stat1.iv()], scale=-0.5)
    stt_(stat1.ap[:, :, 15], stat1.ap[:, :, 12], -1.0, stat1.ap[:, :, 14], ALU.mult, ALU.mult, [stat1.iv()], [stat1.iv()])
    yo = yo + [carve([128, D], F32, at=D0 + 36864 + i * 4096) for i in range(2)]
    for t in range(16):
        a_ap = acc.ap[:, t, :]
        yo_ = yo[t % 4]
        act(yo_.ap, a_ap, AF.Identity, [acc.ivi(t), stat1.iv()], [yo_.iv()], bias=stat1.ap[:, t, 15:16], scale=stat1.ap[:, t, 14:15])
        for eng_, c0_, c1_ in (("dve", 0, 576), ("pool", 576, 1024)):
            tt(eng_, yo_.ap[:, c0_:c1_], yo_.ap[:, c0_:c1_], gb2.ap[:, 0, c0_:c1_], ALU.mult, [yo_.iv(c0_, c1_), gb2.ivi(0)], [yo_.iv(c0_, c1_)])
            tt(eng_, yo_.ap[:, c0_:c1_], yo_.ap[:, c0_:c1_], gb2.ap[:, 1, c0_:c1_], ALU.add, [yo_.iv(c0_, c1_), gb2.ivi(1)], [yo_.iv(c0_, c1_)])
        dma("sp", y[t * 128:(t + 1) * 128, :], yo_.ap, [yo_.iv()], [("y", t, t + 1)], lane="yo%d" % (t % 4))

    P.emit()
    return nc


def _pack_experts(w1, w3, w2):
    a = np.concatenate([w1.reshape(NE, 8, 128, 256), w3.reshape(NE, 8, 128, 256)], axis=3)
    a = a.transpose(0, 2, 1, 3).reshape(NE, 128, 4096)
    b2 = w2.reshape(NE, 2, 128, D).transpose(0, 2, 1, 3).reshape(NE, 128, 2048)
    return np.ascontiguousarray(np.concatenate([a, b2], axis=2).reshape(NE * 128 * 3, 2048))


_NC = None


def kernel(x, mem, ln0_g, ln0_b, rel_bias, w_in, w_mem_kv, w_fourier, lambda_q1, lambda_k1, lambda_q2, lambda_k2,
           subln_g, w_out, ln1_g, ln1_b, w_group, b_group, w_router, b_router, w1, w3, w2, ln2_g, ln2_b):
    global _NC
    f = lambda a: np.ascontiguousarray(np.asarray(a, dtype=np.float32))
    x = f(x)
    mem = f(mem)
    shared = {
        "g0T": f(f(ln0_g).reshape(8, 128).T), "b0T": f(f(ln0_b).reshape(8, 128).T),
        "ln0_g": f(ln0_g), "ln0_b": f(ln0_b), "rel_bias": f(rel_bias), "w_in": f(w_in)[0], "w_mem": f(w_mem_kv)[0],
        "w_four": f(w_fourier)[0], "lq1": f(lambda_q1)[0], "lk1": f(lambda_k1)[0], "lq2": f(lambda_q2)[0], "lk2": f(lambda_k2)[0],
        "subln": f(subln_g)[0], "w_out": f(w_out)[0], "ln1_g": f(ln1_g)[0], "ln1_b": f(ln1_b)[0],
        "wr_cat": f(np.concatenate([f(w_group)[0], f(w_router)[0]], axis=1)),
        "b_cat": f(np.concatenate([f(b_group)[0], f(b_router)[0]], axis=0)),
        "wp": _pack_experts(f(w1)[0], f(w3)[0], f(w2)[0]), "ln2_g": f(ln2_g)[0], "ln2_b": f(ln2_b)[0],
    }
    consts = [_host_consts(0), _host_consts(1)]
    in_maps = []
    for c in range(8):
        b, half = c // 2, c % 2
        m = dict(shared)
        m["x"] = np.ascontiguousarray(np.roll(x[b], -OWN * half, axis=0))
        m["mem"] = mem[b]
        m.update(consts[half])
        in_maps.append(m)
    if _NC is None:
        _NC = build_nc()
    res = run_bass_kernel_spmd(_NC, in_maps, core_ids=list(range(8)))
    out = np.empty((4, S, D), np.float32)
    for c in range(8):
        b, half = c // 2, c % 2
        out[b, half * OWN:(half + 1) * OWN, :] = res.results[c]["y"]
    return out
```

```python
import math
import numpy as np
import ml_dtypes
import concourse.bass as bass
import concourse.mybir as mybir
from concourse.bass_utils import run_bass_kernel_spmd

F32 = mybir.dt.float32
BF16 = mybir.dt.bfloat16
U8 = mybir.dt.uint8
I32 = mybir.dt.int32
U32 = mybir.dt.uint32
AF = mybir.ActivationFunctionType
ALU = mybir.AluOpType
AX = mybir.AxisListType

S = 4096
OWN = 2048
D = 1024
NE = 32
ALPHA = 2.0 ** 0.25
EPS = 1e-5
LAM_INIT = 0.2
GW = 1280
HW = 1152
SBUF_BYTES = 207 * 1024


class Prog:
    BK = 2048

    def __init__(self, nc):
        self.nc = nc
        self.ops = []
        self.lanes = {}
        self.bk = {}

    def _buckets(self, sp, lo, hi):
        return [(sp, b) for b in range(lo // self.BK, (hi - 1) // self.BK + 1)]

    def add(self, eng, fn, reads=(), writes=(), lane=None, group=False):
        idx = len(self.ops)
        deps = set()
        reads = [(sp, lo // 2048 * 2048, (hi + 2047) // 2048 * 2048) if sp == "ps" else (sp, lo, hi) for (sp, lo, hi) in reads]
        writes = [(sp, lo // 2048 * 2048, (hi + 2047) // 2048 * 2048) if sp == "ps" else (sp, lo, hi) for (sp, lo, hi) in writes]
        for (sp, lo, hi) in reads:
            for key in self._buckets(sp, lo, hi):
                for a in self.bk.get(key, ()):
                    if a[0] < hi and lo < a[1] and (a[3] or (sp == "ps" and self.ops[a[2]]["eng"] != eng)):
                        deps.add(a[2])
        for (sp, lo, hi) in writes:
            for key in self._buckets(sp, lo, hi):
                for a in self.bk.get(key, ()):
                    if a[0] < hi and lo < a[1]:
                        deps.add(a[2])
        deps.discard(idx)
        if lane is not None and group:
            deps = {d for d in deps if self.ops[d]["lane"] != lane}
        op = dict(eng=eng, fn=fn, deps=deps, lane=lane)
        self.ops.append(op)
        for (sp, lo, hi) in writes:
            for key in self._buckets(sp, lo, hi):
                L = self.bk.setdefault(key, [])
                L[:] = [a for a in L if not (lo <= a[0] and a[1] <= hi)]
                L.append([lo, hi, idx, True])
        for (sp, lo, hi) in reads:
            for key in self._buckets(sp, lo, hi):
                L = self.bk.setdefault(key, [])
                if lane is None:
                    L[:] = [a for a in L if not ((not a[3]) and a[0] == lo and a[1] == hi
                                                 and self.ops[a[2]]["eng"] == eng and self.ops[a[2]]["lane"] is None)]
                L.append([lo, hi, idx, False])
        if lane is not None:
            Ln = self.lanes.setdefault(lane, dict(group=group, count=0))
            Ln["count"] += 1
            op["lane_seq"] = Ln["count"]
        return idx

    def emit(self):
        nc = self.nc
        ops = self.ops
        engs = ["pe", "act", "dve", "pool", "sp"]
        for i, op in enumerate(ops):
            best = {}
            keep = set()
            for d in op["deps"]:
                pd = ops[d]
                if pd["lane"] is not None:
                    keep.add(d)
                    continue
                if pd["eng"] == "pe" and op["eng"] == "pe" and op["lane"] is None:
                    continue
                if pd["eng"] not in best or best[pd["eng"]] < d:
                    best[pd["eng"]] = d
            keep.update(best.values())
            op["deps"] = keep
        needed = [False] * len(ops)
        for op in ops:
            for d in op["deps"]:
                needed[d] = True
        sems = {e: nc.alloc_semaphore(name="sem_" + e) for e in engs}
        lane_sems = {l: nc.alloc_semaphore(name="lane_%d" % k) for k, l in enumerate(self.lanes)}
        cnt = {e: 0 for e in engs}
        for i, op in enumerate(ops):
            if op["lane"] is not None:
                Ln = self.lanes[op["lane"]]
                v = 16 * (Ln["count"] if Ln["group"] else op["lane_seq"])
                op["sig"] = (lane_sems[op["lane"]], v)
            elif needed[i]:
                cnt[op["eng"]] += 1
                op["sig"] = (sems[op["eng"]], cnt[op["eng"]])
            else:
                op["sig"] = None
        by_eng = {e: [] for e in engs}
        for i, op in enumerate(ops):
            by_eng[op["eng"]].append(i)
        lanes_by_eng = {e: set() for e in engs}
        for op in ops:
            if op["lane"] is not None:
                lanes_by_eng[op["eng"]].add(op["lane"])
        block_cm = nc.Block()
        block = block_cm.__enter__()

        def run_engine(e, handle):
            waited = {}
            for i in by_eng[e]:
                op = ops[i]
                need = {}
                for d in op["deps"]:
                    s, v = ops[d]["sig"]
                    key = id(s)
                    if key not in need or need[key][1] < v:
                        need[key] = (s, v)
                for key, (s, v) in need.items():
                    if waited.get(key, -1) >= v:
                        continue
                    handle.wait_ge(s, v)
                    waited[key] = v
                inst = op["fn"](handle)
                if op["lane"] is not None:
                    inst.then_inc(op["sig"][0], 16)
                elif op["sig"] is not None:
                    inst.then_inc(op["sig"][0], 1)
            for l in sorted(lanes_by_eng[e]):
                handle.wait_ge(lane_sems[l], 16 * self.lanes[l]["count"])

        @block.tensor
        def _(h):
            run_engine("pe", h)

        @block.scalar
        def _(h):
            run_engine("act", h)

        @block.vector
        def _(h):
            run_engine("dve", h)

        @block.gpsimd
        def _(h):
            run_engine("pool", h)

        @block.sync
        def _(h):
            run_engine("sp", h)

        block_cm.__exit__(None, None, None)


class SB:
    def __init__(self, big, off, shape, dt):
        self.esz = 4 if dt in (F32, I32, U32) else 2
        self.n = int(np.prod(shape[1:]))
        self.off = off
        self.shape = shape
        ap = big[:, off:off + self.n * self.esz].bitcast(dt)
        if len(shape) == 3:
            ap = ap.rearrange("p (a b) -> p a b", a=shape[1])
        elif len(shape) == 4:
            ap = ap.rearrange("p (a b c) -> p a b c", a=shape[1], b=shape[2])
        self.ap = ap

    def iv(self, lo=0, hi=None):
        if hi is None:
            hi = self.n
        return ("sb", self.off + lo * self.esz, self.off + hi * self.esz)

    def ivi(self, *idx):
        stride = self.n
        lo = 0
        for k, i in enumerate(idx):
            stride //= self.shape[1 + k]
            lo += i * stride
        return self.iv(lo, lo + stride)


def PSI(bank, lo=0, hi=512, nb=1):
    return ("ps", bank * 2048 + lo * 4, (bank + nb - 1) * 2048 + hi * 4)


def _t5_bucket(rel):
    nb = 16
    max_exact = 8
    ret = (rel > 0).astype(np.int64) * nb
    n = np.abs(rel)
    nf = np.maximum(n, 1).astype(np.float32)
    large = max_exact + (np.log(nf / np.float32(max_exact)) / np.float32(math.log(128 / max_exact))
                         * np.float32(nb - max_exact)).astype(np.int32)
    large = np.minimum(large, nb - 1)
    return ret + np.where(n < max_exact, n, large)


def _band_kbs(qg):
    return [(kb % 32) for kb in range(4 * qg - 1, 4 * qg + 5)]


def _host_consts(half):
    c = {}
    c["identb"] = np.eye(128, dtype=np.float32).astype(ml_dtypes.bfloat16)
    c["identf"] = np.eye(128, dtype=np.float32)
    c["jb"] = np.eye(128, dtype=np.float32)[::-1].copy().astype(ml_dtypes.bfloat16)
    n = np.arange(GW)
    bk = _t5_bucket(639 - n)
    oh = np.zeros((32, GW), np.float32)
    oh[bk, n] = 1.0
    oh2 = np.zeros((32, GW), np.float32)
    if half == 0:
        oh2[:, :640] = oh[:, :640]
        oh2[31, 640:] = 1.0
    else:
        oh2[15, :640] = 1.0
        oh2[:, 640:] = oh[:, 640:]
    c["oh"] = oh
    c["oh2"] = oh2
    selb = np.zeros((128,), np.float32)
    for qg in range(4):
        band = _band_kbs(qg)
        for kb in range(32):
            if kb in band:
                continue
            if kb < 16:
                before = kb < 4 * qg - 1
            else:
                before = (half == 1)
            selb[qg * 32 + kb] = 1.0 if before else 0.0
    c["selb"] = selb
    selb2 = np.zeros((256,), np.float32)
    for g in range(8):
        band2 = [(kb % 32) for kb in range(2 * g - 1, 2 * g + 3)]
        for kb in range(32):
            if kb in band2:
                continue
            if kb < 16:
                before = kb < 2 * g - 1
            else:
                before = (half == 1)
            selb2[g * 32 + kb] = 1.0 if before else 0.0
    c["selb2"] = selb2
    k = np.arange(64)
    ang = 2.0 * np.pi * ((k[:, None] * k[None, :]) % 64) / 64.0
    c64 = (np.cos(ang) / 512.0).astype(np.float32)
    s64 = (np.sin(ang) / 512.0).astype(np.float32)
    z = np.zeros((64, 64), np.float32)
    c["c64bd"] = np.block([[c64, z], [z, c64]])
    c["s64bd"] = np.block([[s64, z], [z, s64]])
    pt = (np.arange(S) + OWN * half) % S
    ps = np.arange(OWN) + OWN * half
    idx = (pt[:, None] * ps[None, :]) % S
    tab = 2.0 * np.pi * np.arange(S) / S
    ct = np.cos(tab).astype(np.float32).astype(ml_dtypes.bfloat16)
    st = (-np.sin(tab)).astype(np.float32).astype(ml_dtypes.bfloat16)
    dft = np.empty((2, 4, S, 512), ml_dtypes.bfloat16)
    for sg in range(4):
        sub = idx[:, sg * 512:(sg + 1) * 512]
        dft[0, sg] = ct[sub]
        dft[1, sg] = st[sub]
    c["dft"] = dft
    c["u_tri"] = np.triu(np.ones((128, 128), np.float32), 1)
    c["ones_c"] = np.ones((128, 128), np.float32)
    c["tau_c"] = np.arange(64, dtype=np.float32)
    pj = np.zeros((128, 4), np.float32)
    pj[:, 0:3] = 3.0 * np.arange(128, dtype=np.float32)[:, None] + np.arange(3, dtype=np.float32)[None, :]
    pj[:, 3] = np.arange(128, dtype=np.float32)
    c["pj_c"] = pj
    c["thr_c"] = np.tile(128.0 * np.arange(16, dtype=np.float32)[None, :], (32, 1)).reshape(512)
    return c


def build_nc(stop=None):
    nc = bass.Bass("TRN2", target_bir_lowering=False)

    def finish(dumps):
        for name, sbuf in dumps:
            shp = [128, sbuf.n]
            dt = F32 if sbuf.esz == 4 else BF16
            d = nc.dram_tensor("dbg_" + name, shp, dt, kind="ExternalOutput").ap()
            flat = big[:, sbuf.off:sbuf.off + sbuf.n * sbuf.esz].bitcast(dt)
            P.add("sp", lambda e, d=d, flat=flat: e.dma_start(out=d, in_=flat), reads=[sbuf.iv()], writes=[("dbg_" + name, 0, 1)],
                  lane="dbg_" + name)
        P.emit()
        return nc

    def din(name, shape, dt=F32):
        return nc.dram_tensor(name, list(shape), dt, kind="ExternalInput").ap()

    x = din("x", [S, D])
    mem = din("mem", [256, D])
    g0T = din("g0T", [128, 8])
    b0T = din("b0T", [128, 8])
    ln0_g = din("ln0_g", [D])
    ln0_b = din("ln0_b", [D])
    rel_bias = din("rel_bias", [32, 4])
    w_in = din("w_in", [D, 2048])
    w_mem = din("w_mem", [D, 512])
    w_four = din("w_four", [4, 64, 64])
    lq1 = din("lq1", [64])
    lk1 = din("lk1", [64])
    lq2 = din("lq2", [64])
    lk2 = din("lk2", [64])
    subln = din("subln", [128])
    w_out = din("w_out", [D, D])
    ln1_g = din("ln1_g", [D])
    ln1_b = din("ln1_b", [D])
    wr_cat = din("wr_cat", [D, 36])
    b_cat = din("b_cat", [36])
    wp_d = din("wp", [NE * 128 * 3, 2048])
    u_d = din("u_tri", [128, 128])
    ones_d = din("ones_c", [128, 128])
    tau_d = din("tau_c", [64])
    pj_d = din("pj_c", [128, 4])
    thr_d = din("thr_c", [512])
    ln2_g = din("ln2_g", [D])
    ln2_b = din("ln2_b", [D])
    identb_d = din("identb", [128, 128], BF16)
    identf_d = din("identf", [128, 128])
    jb_d = din("jb", [128, 128], BF16)
    oh_d = din("oh", [32, GW])
    oh2_d = din("oh2", [32, GW])
    selb_d = din("selb", [128])
    selb2_d = din("selb2", [256])
    c64_d = din("c64bd", [128, 128])
    s64_d = din("s64bd", [128, 128])
    dft = din("dft", [2, 4, S, 512], BF16)
    gscr = nc.dram_tensor("gscr", [8, GW], F32, kind="Internal").ap()
    xs_z = nc.dram_tensor("xs_scr", [64 * 128, D + 4], BF16, kind="Internal").ap()
    y = nc.dram_tensor("y", [OWN, D], F32, kind="ExternalOutput").ap()

    big = nc.sbuf_tensor("big", [128, SBUF_BYTES], U8).__enter__()
    ps = nc.psum_tensor("ps", [128, 8, 512], F32).__enter__()

    def psb(bank):
        return ps[:, bank, :].bitcast(BF16)

    P = Prog(nc)
    cur = [0]

    def carve(shape, dt, at=None):
        esz = 4 if dt in (F32, I32, U32) else 2
        nbytes = int(np.prod(shape[1:])) * esz
        if at is None:
            off = cur[0]
            cur[0] = (off + nbytes + 63) // 64 * 64
            assert cur[0] <= SBUF_BYTES, ("sbuf overflow", cur[0])
        else:
            off = at
        return SB(big, off, shape, dt)

    kT = carve([128, 4, S], BF16)
    Vaug = carve([128, 32, 4, 130], BF16)
    qT = carve([128, 4, OWN], BF16)
    utok = carve([128, 32, 256], BF16)
    mqT = carve([128, 2, OWN], BF16)
    identb = carve([128, 128], BF16)
    identf = carve([128, 128], F32)
    jb = carve([128, 128], BF16)
    tabB = carve([128, 128], F32)
    selb = carve([128, 128], F32)
    cb = carve([128, 4, 128], F32)
    g0Ts = carve([128, 8], F32)
    b0Ts = carve([128, 8], F32)
    stat0 = carve([128, 16, 2], F32)
    lamb = carve([128, 4, 64], F32)
    small = carve([128, 64], F32)
    gsub = carve([128, 128], F32)
    stt = carve([128, 8, 16], F32)
    mkT = carve([128, 2, 256], BF16)
    mvaug = carve([128, 2, 4, 66], BF16)
    Hb = carve([128, 4, HW], BF16)
    H2b = carve([128, 4, HW], BF16)
    Hst = carve([128, GW], F32)
    ABD = carve([128, 2, 256], BF16)
    tab32 = carve([128, 8], F32)
    tabP = carve([128, 128], F32)
    ohs = carve([128, GW], F32)
    R1 = cur[0]
    cur[0] += 32768
    R2 = cur[0]
    cur[0] += 32768
    assert cur[0] <= SBUF_BYTES, cur[0]
    w_inb = carve([128, 8, 2048], BF16, at=R1)
    h0T = [carve([128, 8, 512], BF16, at=R2 + i * 8192) for i in range(2)]
    xt = [carve([128, D], F32, at=R2 + 16384 + i * 4096) for i in range(2)]
    xn = [carve([128, D], BF16, at=R2 + 24576 + i * 2048) for i in range(4)]
    wmem = carve([128, 8, 512], BF16, at=R2)
    memf = carve([128, 2, D], F32, at=R2 + 8192)
    membf = carve([128, 2, D], BF16, at=R2 + 24576)
    memT = carve([128, 8, 256], BF16, at=R2 + 28672)
    wbd = carve([128, 2, 128], F32, at=R1 + 24576)
    ring = [carve([128, 8, 512], BF16, at=R1 + i * 8192) for i in range(3)]
    ybuf = carve([128, 4, 512], BF16, at=R1 + 24576)
    oT = carve([128, 8, OWN], BF16, at=R2)
    PT2 = [carve([128, 2, 512], BF16, at=R1 + 8192 + i * 2048) for i in range(3)]
    PT = [[carve([128, 512], BF16, at=R1 + 8192 + i * 2048 + c * 1024) for i in range(3)] for c in range(2)]
    accS = [carve([128, 264], F32, at=R1 + 16384 + i * 1088) for i in range(2)]
    post = carve([128, 8, 128], F32, at=R1 + 20480)

    def dma(eng, out_ap, in_ap, reads, writes, lane, group=False):
        P.add(eng, lambda e: e.dma_start(out=out_ap, in_=in_ap), reads=reads, writes=writes, lane=lane, group=group)

    def mm(out_ap, lhsT, rhs, start, stop, reads, writes, skip=False):
        if skip:
            P.add("pe", lambda e: e.matmul(out_ap, lhsT=lhsT, rhs=rhs, start=start, stop=stop, skip_group_check=True),
                  reads=reads, writes=writes)
        else:
            P.add("pe", lambda e: e.matmul(out_ap, lhsT=lhsT, rhs=rhs, start=start, stop=stop), reads=reads, writes=writes)

    def tr(out_ap, in_ap, ident, reads, writes):
        P.add("pe", lambda e: e.transpose(out=out_ap, in_=in_ap, identity=ident), reads=reads, writes=writes)

    def act(out_ap, in_ap, func, reads, writes, bias=None, scale=None, accum=None):
        kw = {}
        if bias is not None:
            kw["bias"] = bias
        if scale is not None:
            kw["scale"] = scale
        if accum is not None:
            kw["accum_out"] = accum
        P.add("act", lambda e: e.activation(out=out_ap, in_=in_ap, func=func, **kw), reads=reads, writes=writes)

    def ts(eng, out_ap, in_ap, s1, s2, op0, op1, reads, writes):
        if op1 is None:
            P.add(eng, lambda e: e.tensor_scalar(out=out_ap, in0=in_ap, scalar1=s1, scalar2=None, op0=op0), reads=reads, writes=writes)
        else:
            P.add(eng, lambda e: e.tensor_scalar(out=out_ap, in0=in_ap, scalar1=s1, scalar2=s2, op0=op0, op1=op1), reads=reads, writes=writes)

    def tt(eng, out_ap, a, b, op, reads, writes):
        P.add(eng, lambda e: e.tensor_tensor(out=out_ap, in0=a, in1=b, op=op), reads=reads, writes=writes)

    def stt_(out_ap, in0, scalar, in1, op0, op1, reads, writes, accum=None):
        if accum is None:
            P.add("dve", lambda e: e.scalar_tensor_tensor(out=out_ap, in0=in0, scalar=scalar, in1=in1, op0=op0, op1=op1),
                  reads=reads, writes=writes)
        else:
            P.add("dve", lambda e: e.scalar_tensor_tensor(out=out_ap, in0=in0, scalar=scalar, in1=in1, op0=op0, op1=op1,
                                                          accum_out=accum), reads=reads, writes=writes)

    def cp(eng, out_ap, in_ap, reads, writes):
        P.add(eng, lambda e: e.tensor_copy(out=out_ap, in_=in_ap), reads=reads, writes=writes)

    def memset(eng, ap, val, writes):
        P.add(eng, lambda e: e.memset(ap, val), writes=writes)

    G = dict(lane="c", group=True)
    G0 = dict(lane="c0", group=True)
    for t_ in range(2):
        dma("sp", xt[t_].ap, x[t_ * 128:(t_ + 1) * 128, :], [], [xt[t_].iv()], lane="x%d" % t_)
    dma("sp", identb.ap, identb_d, [], [identb.iv()], **G0)
    dma("sp", g0Ts.ap, g0T, [], [g0Ts.iv()], **G0)
    dma("sp", b0Ts.ap, b0T, [], [b0Ts.iv()], **G0)
    dma("sp", identf.ap, identf_d, [], [identf.iv()], **G)
    dma("sp", jb.ap, jb_d, [], [jb.iv()], **G)
    dma("sp", tabB.ap, rel_bias.rearrange("a b -> (a b)").partition_broadcast(128), [], [tabB.iv()], **G)
    dma("sp", selb.ap, selb_d.partition_broadcast(128), [], [selb.iv()], **G)
    for i, v in enumerate([lq1, lk1, lq2, lk2]):
        dma("sp", lamb.ap[:, i, :], v.partition_broadcast(128), [], [lamb.ivi(i)], **G)
    dma("sp", gsub.ap, subln.partition_broadcast(128), [], [gsub.iv()], **G)
    dma("sp", tab32.ap[0:32, 0:4], rel_bias, [], [tab32.iv(0, 4)], **G)
    dma("sp", tab32.ap[0:32, 4:8], rel_bias[31, :].partition_broadcast(32), [], [tab32.iv(4, 8)], **G)
    memset("pool", ohs.ap, 0.0, [ohs.iv()])
    memset("pool", tabP.ap, 0.0, [tabP.iv()])
    dma("sp", ohs.ap[0:32, :], oh_d, [], [ohs.iv()], lane="oh1")
    if stop == "c0":
        return finish([("small", small), ("gsub", gsub)])
    w_in_v = w_in.rearrange("(k p) n -> p k n", p=128)
    for cbk in (1, 0, 3, 2):
        c0 = cbk * 512
        dma("pool", w_inb.ap[:, :, c0:c0 + 512], w_in_v[:, :, c0:c0 + 512], [],
            [w_inb.iv(k * 2048 + c0, k * 2048 + c0 + 512) for k in range(8)], lane="w_in%d" % cbk)
    if stop == "c0w":
        return finish([("small", small), ("gsub", gsub)])
    Vaug3 = carve([128, 128, 130], BF16, at=Vaug.off)
    mvaug3 = carve([128, 8, 66], BF16, at=mvaug.off)
    memset("pool", Vaug3.ap[:, :, 128:130], 1.0, [Vaug.iv()])
    memset("pool", mvaug3.ap[:, :, 64:66], 1.0, [mvaug.iv()])
    if stop == "c1":
        return finish([("small", small), ("gsub", gsub)])

    sm = small.ap
    junk = Hst.ap[:, 0:64]
    stt_(junk, lamb.ap[:, 0, :], 1.0, lamb.ap[:, 1, :], ALU.mult, ALU.mult, [lamb.ivi(0), lamb.ivi(1)],
         [Hst.iv(), small.iv(0, 1)], accum=sm[:, 0:1])
    stt_(junk, lamb.ap[:, 2, :], 1.0, lamb.ap[:, 3, :], ALU.mult, ALU.mult, [lamb.ivi(2), lamb.ivi(3)],
         [Hst.iv(), small.iv(1, 2)], accum=sm[:, 1:2])
    act(sm[:, 2:4], sm[:, 0:2], AF.Exp, [small.iv(0, 2)], [small.iv(2, 4)])
    tt("dve", sm[:, 4:5], sm[:, 3:4], sm[:, 2:3], ALU.subtract, [small.iv(2, 4)], [small.iv(4, 5)])
    ts("dve", sm[:, 4:5], sm[:, 4:5], -LAM_INIT, None, ALU.add, None, [small.iv(4, 5)], [small.iv(4, 5)])
    ts("dve", gsub.ap, gsub.ap, 1.0 - LAM_INIT, None, ALU.mult, None, [gsub.iv()], [gsub.iv()])
    tt("dve", tabP.ap[0:32, 0:4], tab32.ap[0:32, 0:4], tab32.ap[0:32, 4:8], ALU.subtract, [tab32.iv(), tabP.iv()], [tabP.iv()])
    for h in range(4):
        tt("dve", sm[:, 8 + h:9 + h], tabB.ap[:, 60 + h:61 + h], tabB.ap[:, 124 + h:125 + h], ALU.subtract,
           [tabB.iv()], [small.iv(8 + h, 9 + h)])
        ts("dve", cb.ap[:, h, :], selb.ap, sm[:, 8 + h:9 + h], None, ALU.mult, None, [selb.iv(), small.iv(8 + h, 9 + h)], [cb.ivi(h)])
    if stop == "const":
        return finish([("Hb", Hb), ("H2b", H2b), ("cb", cb), ("small", small), ("gsub", gsub)])
    rr = [0]

    def evac_eng():
        rr[0] += 1
        return "act" if rr[0] % 2 else "dve"

    def evac(out_ap, in_ap, reads, writes, scale=None, eng=None):
        e = eng or evac_eng()
        if e == "act":
            if scale is None:
                act(out_ap, in_ap, AF.Copy, reads, writes)
            else:
                act(out_ap, in_ap, AF.Copy, reads, writes, scale=scale)
        else:
            if scale is None:
                cp("dve", out_ap, in_ap, reads, writes)
            else:
                ts("dve", out_ap, in_ap, scale, None, ALU.mult, None, reads, writes)

    def ln_stats(src_ap, src_iv, slot, rstd_ap, rstd_iv, nb_ap, nb_iv):
        s = stt.ap[:, slot, :]
        siv = stt.ivi(slot)
        P.add("dve", lambda e: e.bn_stats(out=s[:, 0:6], in_=src_ap[:, 0:512]), reads=[src_iv], writes=[siv])
        P.add("dve", lambda e: e.bn_stats(out=s[:, 6:12], in_=src_ap[:, 512:1024]), reads=[src_iv], writes=[siv])
        P.add("dve", lambda e: e.bn_aggr(out=s[:, 12:14], in_=s[:, 0:12]), reads=[siv], writes=[siv])
        act(s[:, 14:15], s[:, 13:14], AF.Ln, [siv], [siv], bias=EPS, scale=1.0)
        act(rstd_ap, s[:, 14:15], AF.Exp, [siv], [rstd_iv], scale=-0.5)
        stt_(nb_ap, s[:, 12:13], -1.0, rstd_ap, ALU.mult, ALU.mult, [siv, rstd_iv], [nb_iv])

    pbank = [4]

    def next_bank(lo=4, hi=8):
        b = pbank[0]
        pbank[0] = lo + (pbank[0] - lo + 1) % (hi - lo)
        return b

    def ln_tile(t):
        xb_ = xt[t % 2]
        if t >= 2:
            dma("sp", xb_.ap, x[t * 128:(t + 1) * 128, :], [], [xb_.iv()], lane="x%d" % (t % 2))
        slot = t % 8
        if t < 16:
            r_ap, r_iv = stat0.ap[:, t, 0:1], stat0.iv(2 * t, 2 * t + 1)
            n_ap, n_iv = stat0.ap[:, t, 1:2], stat0.iv(2 * t + 1, 2 * t + 2)
        else:
            r_ap, r_iv = stt.ap[:, slot, 14:15], stt.ivi(slot)
            n_ap, n_iv = stt.ap[:, slot, 15:16], stt.ivi(slot)
        ln_stats(xb_.ap, xb_.iv(), slot, r_ap, r_iv, n_ap, n_iv)
        xnb = xn[t % 4]
        act(xnb.ap, xb_.ap, AF.Identity, [xb_.iv(), r_iv, n_iv], [xnb.iv()], bias=n_ap, scale=r_ap)

    def tr_tile(t):
        Gi, tl = t // 4, t % 4
        hb = h0T[Gi % 2]
        xnb = xn[t % 4]
        tb = 2 * (t % 2)
        for k in range(8):
            tr(psb(tb + k // 4)[:, (k % 4) * 128:(k % 4 + 1) * 128], xnb.ap[:, k * 128:(k + 1) * 128], identb.ap,
               [xnb.iv(), identb.iv()], [PSI(tb + k // 4)])
        for k in range(8):
            src = psb(tb + k // 4)[:, (k % 4) * 128:(k % 4 + 1) * 128]
            dst = hb.ap[:, k, tl * 128:(tl + 1) * 128]
            div = hb.iv(k * 512 + tl * 128, k * 512 + tl * 128 + 128)
            if k < 4:
                ts("dve", dst, src, g0Ts.ap[:, k:k + 1], b0Ts.ap[:, k:k + 1], ALU.mult, ALU.add,
                   [PSI(tb), g0Ts.iv(), b0Ts.iv()], [div])
            else:
                act(dst, src, AF.Identity, [PSI(tb + 1), g0Ts.iv(), b0Ts.iv()], [div],
                    bias=b0Ts.ap[:, k:k + 1], scale=g0Ts.ap[:, k:k + 1])

    def proj_units(Gi):
        hb = h0T[Gi % 2]
        c0, c1 = Gi * 512, Gi * 512 + 512
        units = []

        def fm(col0, dst_ap, dst_iv, scale=None):
            def u():
                b = next_bank()
                for k in range(8):
                    mm(ps[:, b, :], w_inb.ap[:, k, col0:col0 + 128], hb.ap[:, k, :], k == 0, k == 7,
                       [w_inb.iv(k * 2048 + col0, k * 2048 + col0 + 128), hb.ivi(k)], [PSI(b)])
                evac(dst_ap, ps[:, b, :], [PSI(b)], [dst_iv], scale=scale)
            return u

        for h in range(4):
            units.append(fm(512 + 128 * h, kT.ap[:, h, c0:c1], kT.iv(h * S + c0, h * S + c1)))
        if Gi < 4:
            for h in range(4):
                units.append(fm(128 * h, qT.ap[:, h, c0:c1], qT.iv(h * OWN + c0, h * OWN + c1), 0.125))
            for j in range(2):
                units.append(fm(1792 + 128 * j, mqT.ap[:, j, c0:c1], mqT.iv(j * OWN + c0, j * OWN + c1), 0.125))
        for tl in range(4):
            t = Gi * 4 + tl

            def uv(t=t, tl=tl):
                b = next_bank()
                for k in range(8):
                    mm(ps[:, b, :], hb.ap[:, k, tl * 128:(tl + 1) * 128], w_inb.ap[:, k, 1024:1536], k == 0, k == 7,
                       [w_inb.iv(k * 2048 + 1024, k * 2048 + 1536), hb.ivi(k)], [PSI(b)])
                evac(Vaug.ap[:, t, :, 0:128], ps[:, b, :].rearrange("p (a b) -> p a b", a=4), [PSI(b)], [Vaug.ivi(t)])

            def uu(t=t, tl=tl):
                b = next_bank()
                for k in range(8):
                    mm(ps[:, b, 0:256], hb.ap[:, k, tl * 128:(tl + 1) * 128], w_inb.ap[:, k, 1536:1792], k == 0, k == 7,
                       [w_inb.iv(k * 2048 + 1536, k * 2048 + 1792), hb.ivi(k)], [PSI(b, 0, 256)])
                evac(utok.ap[:, t, :], ps[:, b, 0:256], [PSI(b, 0, 256)], [utok.ivi(t)])
            units.append(uv)
            units.append(uu)
        return units

    for tl in range(4):
        ln_tile(tl)
    for tl in range(4):
        tr_tile(tl)
    for Gi in range(8):
        units = proj_units(Gi)
        nxt = [(Gi + 1) * 4 + tl for tl in range(4)] if Gi < 7 else []
        for t in nxt:
            ln_tile(t)
        nu = len(units)
        q = (nu + 3) // 4
        for j in range(4):
            for u in units[j * q:(j + 1) * q]:
                u()
            if nxt:
                tr_tile(nxt[j])

    def build_H(var, ohd, Hdst):
            if var == 1:
                dma("sp", ohs.ap[0:32, :], ohd, [], [ohs.iv()], lane="oh2")
            for j in range(3):
                n0 = j * 512
                n1 = min(GW, n0 + 512)
                mm(ps[:, j, 0:n1 - n0], tabP.ap, ohs.ap[:, n0:n1], True, True,
                   [tabP.iv(), ohs.iv()], [PSI(j)])
                cp("dve", Hst.ap[0:4, n0:n1], ps[0:4, j, 0:n1 - n0], [PSI(j)], [Hst.iv()])
            dma("sp", gscr[var * 4:var * 4 + 4, :], Hst.ap[0:4, 0:GW], [Hst.iv()], [("gscr", var, var + 1)], lane="gs%d" % var)
            for h in range(4):
                hank = bass.AP(tensor=gscr.tensor, offset=gscr[var * 4 + h, 0:1].offset, ap=[[1, 128], [1, HW]])
                dma("pool", Hdst.ap[:, h, :], hank, [("gscr", var, var + 1)], [Hdst.ivi(h)], lane="hk%d%d" % (var, h))

    build_H(0, None, Hb)
    if stop == "A":
        return finish([("kT", kT), ("qT", qT), ("Vaug", Vaug), ("utok", utok), ("mqT", mqT), ("stat0", stat0)])
    dma("pool", wmem.ap, w_mem.rearrange("(k p) n -> p k n", p=128), [], [wmem.iv()], lane="wmem")
    dma("sp", memf.ap, mem.rearrange("(t p) d -> p t d", p=128), [], [memf.iv()], lane="memf")
    memset("dve", wbd.ap, 0.0, [wbd.iv()])
    for g in range(4):
        j, r = g // 2, g % 2
        dma("sp", wbd.ap[64 * r:64 * r + 64, j, 64 * r:64 * r + 64], w_four[g], [], [wbd.iv()], lane="wbd", group=True)
    c64s = carve([128, 128], F32, at=R1 + 26624)
    s64s = carve([128, 128], F32, at=R1 + 27136)
    dma("sp", c64s.ap, c64_d, [], [c64s.iv()], lane="c64s", group=True)
    dma("sp", s64s.ap, s64_d, [], [s64s.iv()], lane="c64s", group=True)
    for j in range(2):
        for q_, cs_ in enumerate((c64s, s64s)):
            b = next_bank()
            mm(ps[:, b, 0:128], cs_.ap, wbd.ap[:, j, :], True, True, [cs_.iv(), wbd.iv()], [PSI(b, 0, 128)])
            evac(ABD.ap[:, j, q_ * 128:(q_ + 1) * 128], ps[:, b, 0:128], [PSI(b, 0, 128)], [ABD.iv(j * 256 + q_ * 128, j * 256 + q_ * 128 + 128)])

    def mem_pe():
        cp("pool", membf.ap, memf.ap, [memf.iv()], [membf.iv()])
        for tm in range(2):
            for k in range(8):
                tr(psb(tm)[:, k * 128:(k + 1) * 128], membf.ap[:, tm, k * 128:(k + 1) * 128], identb.ap,
                   [membf.iv(), identb.iv()], [PSI(tm)])
            evac(memT.ap[:, :, tm * 128:(tm + 1) * 128], psb(tm).rearrange("p (a b) -> p a b", a=8), [PSI(tm)], [memT.iv()])
        for j in range(2):
            b = next_bank()
            for k in range(8):
                mm(ps[:, b, 0:256], wmem.ap[:, k, 128 * j:128 * j + 128], memT.ap[:, k, :], k == 0, k == 7,
                   [wmem.iv(), memT.iv()], [PSI(b, 0, 256)])
            evac(mkT.ap[:, j, :], ps[:, b, 0:256], [PSI(b, 0, 256)], [mkT.ivi(j)])
        for tm in range(2):
            b = next_bank()
            for k in range(8):
                mm(ps[:, b, 0:256], memT.ap[:, k, tm * 128:(tm + 1) * 128], wmem.ap[:, k, 256:512], k == 0, k == 7,
                   [wmem.iv(), memT.iv()], [PSI(b, 0, 256)])
            evac(mvaug.ap[:, tm, :, 0:64], ps[:, b, 0:256].rearrange("p (a b) -> p a b", a=4), [PSI(b, 0, 256)], [mvaug.ivi(tm)])

    slot_i = [0]
    for sg in range(4):
        ybanks = {}
        for cs in range(2):
            pair = ((sg * 2 + cs) % 3) * 2
            for j in range(2):
                ybanks[(cs, j)] = pair + j
            for tq in range(4):
                sl = slot_i[0] % 3
                slot_i[0] += 1
                rg = ring[sl]
                dma("sp", rg.ap, dft[cs, sg, tq * 1024:(tq + 1) * 1024, :].rearrange("(t p) n -> p t n", p=128),
                    [], [rg.iv()], lane="ring%d" % sl)
                for tc in range(8):
                    t = tq * 8 + tc
                    for j in range(2):
                        b = pair + j
                        mm(ps[:, b, :], utok.ap[:, t, 128 * j:128 * j + 128], rg.ap[:, tc, :], t == 0, t == 31,
                           [utok.ivi(t), rg.ivi(tc)], [PSI(b)])
            for j in range(2):
                b = pair + j
                evac(ybuf.ap[:, cs * 2 + j, :], ps[:, b, :], [PSI(b)], [ybuf.ivi(cs * 2 + j)])
        for j in range(2):
            b = 6 + j
            mm(ps[:, b, :], ABD.ap[:, j, 0:128], ybuf.ap[:, j, :], True, False, [ABD.iv(), ybuf.ivi(j)], [PSI(b)])
            mm(ps[:, b, :], ABD.ap[:, j, 128:256], ybuf.ap[:, 2 + j, :], False, True, [ABD.iv(), ybuf.ivi(2 + j)], [PSI(b)])
            evac(oT.ap[:, 4 + j, sg * 512:(sg + 1) * 512], ps[:, b, :], [PSI(b)],
                 [oT.iv((4 + j) * OWN + sg * 512, (4 + j) * OWN + sg * 512 + 512)])
        if sg == 0:
            build_H(1, oh2_d, H2b)
        if sg == 1:
            mem_pe()

    if stop == "four":
        return finish([("oT", oT)])
    selb2 = carve([128, 256], F32, at=ohs.off)
    cb2 = carve([128, 4, 256], F32, at=Hst.off)
    dma("sp", selb2.ap, selb2_d.partition_broadcast(128), [], [selb2.iv()], lane="selb2")
    for h in range(4):
        ts("dve", cb2.ap[:, h, :], selb2.ap, sm[:, 8 + h:9 + h], None, ALU.mult, None, [selb2.iv(), small.iv(8 + h, 9 + h)], [cb2.ivi(h)])
    zt_ = carve([128, D + 4], BF16, at=R1)
    memset("dve", zt_.ap, 0.0, [zt_.iv()])
    for tz in range(64):
        dma("sp", xs_z[tz * 128:(tz + 1) * 128, :], zt_.ap, [zt_.iv()], [("xs", 0, 64)], lane="xsz", group=True)
    wb_d = nc.dram_tensor("wb_scr", [NE * 128 * 3, 2048], BF16, kind="Internal").ap()
    wstg = [carve([128, 2048], BF16, at=R1 + 24576 + i * 4096) for i in range(2)]
    wp_v = wp_d.rearrange("(e p j) n -> e j p n", e=NE, p=128, j=3)
    wb_v = wb_d.rearrange("(e p j) n -> e j p n", e=NE, p=128, j=3)
    ci = 0
    for e_ in range(NE):
        for j in range(3):
            sg_ = wstg[ci % 2]
            dma("pool", sg_.ap, wp_v[e_, j], [], [sg_.iv()], lane="wcv%d" % (ci % 2))
            dma("sp", wb_v[e_, j], sg_.ap, [sg_.iv()], [("wb", 2 + ci, 3 + ci)], lane="wcw%d" % (ci % 2))
            ci += 1
    otok = carve([128, 16, 512], BF16, at=utok.off)
    otokm = carve([128, 16, 256], BF16, at=R1)
    SC = [[0, 2], [1, 3]]
    Qbd = [carve([128, 2, 256], BF16, at=R1 + 14336 + i * 1024) for i in range(2)]
    for i in range(2):
        memset("dve", Qbd[i].ap, 0.0, [Qbd[i].iv()])
    PTs = [[carve([128, 512], BF16, at=R1 + 8192 + (st * 3 + i) * 1024) for i in range(3)] for st in range(2)]

    def band2(g):
        return [(kb % 32) for kb in range(2 * g - 1, 2 * g + 3)]

    def stream_ops(h, g, st):
        q0 = g * 256
        band = band2(g)
        qb_ = Qbd[st]
        sbank = [2 * st, 2 * st + 1]
        abank = [4 + 2 * st, 5 + 2 * st]
        pts = PTs[st]

        def build():
            for c in range(2):
                cp("dve", qb_.ap[64 * c:64 * c + 64, c, :], qT.ap[64 * c:64 * c + 64, h, q0:q0 + 256],
                   [qT.iv(h * OWN + q0, h * OWN + q0 + 256)], [qb_.iv()])

        def qk(kb):
            b = sbank[kb % 2]
            isb = kb in band
            mm(ps[:, b, :], kT.ap[:, h, kb * 128:(kb + 1) * 128], qb_.ap.rearrange("p c q -> p (c q)"), True, not isb,
               [kT.iv(h * S + kb * 128, h * S + kb * 128 + 128), qb_.iv()], [PSI(b)])
            if isb:
                wrap = (g == 0 and kb == 31) or (g == 7 and kb == 16)
                a_ = 256 * g - 128 * kb
                if wrap:
                    a_ = 128 if g == 0 else -256
                j0 = a_ + 512
                Hs = H2b if wrap else Hb
                for c in range(2):
                    mm(ps[:, b, c * 256:(c + 1) * 256], jb.ap, Hs.ap[:, h, j0:j0 + 256], False, c == 1, [jb.iv(), Hs.ivi(h)], [PSI(b)])

        def ex(kb):
            b = sbank[kb % 2]
            pt = pts[kb % 3]
            if (kb in band) or (kb < 16 and kb > 2 * g + 2):
                act(pt.ap, ps[:, b, :], AF.Exp, [PSI(b)], [pt.iv()])
            else:
                pi = g * 32 + kb
                act(pt.ap, ps[:, b, :], AF.Exp, [PSI(b), cb2.ivi(h)], [pt.iv()], bias=cb2.ap[:, h, pi:pi + 1])

        def av(kb):
            pt = pts[kb % 3]
            for qb in range(2):
                for c in range(2):
                    mm(ps[:, abank[qb], c * 130:c * 130 + 129], pt.ap[:, c * 256 + qb * 128:c * 256 + (qb + 1) * 128],
                       Vaug.ap[:, kb, h, 0:129], (kb == 0 and c == 0), (kb == 31),
                       [pt.iv(), Vaug.ivi(kb)], [PSI(abank[qb], c * 130, c * 130 + 129)], skip=True)

        def copies():
            for qb in range(2):
                a_s = accS4[st][qb]
                cp("dve", a_s.ap[:, 0:260], ps[:, abank[qb], 0:260], [PSI(abank[qb], 0, 260)], [a_s.iv()])

        def part1():
            for qb in range(2):
                j = st * 2 + qb
                a_s = accS4[st][qb]
                A = a_s.ap
                pm = psm.ap
                P.add("dve", lambda e, A=A, pm=pm, j=j: e.reciprocal(out=pm[:, j:j + 1], in_=A[:, 128:129]), reads=[a_s.iv()], writes=[psm.iv(j, j + 1)])
                P.add("dve", lambda e, A=A, pm=pm, j=j: e.reciprocal(out=pm[:, 4 + j:5 + j], in_=A[:, 258:259]), reads=[a_s.iv()], writes=[psm.iv(4 + j, 5 + j)])
                tt("dve", pm[:, 4 + j:5 + j], pm[:, 4 + j:5 + j], sm[:, 4:5], ALU.mult, [psm.iv(4 + j, 5 + j), small.iv(4, 5)], [psm.iv(4 + j, 5 + j)])
                ts("dve", A[:, 0:128], A[:, 0:128], pm[:, j:j + 1], None, ALU.mult, None, [a_s.iv(), psm.iv(j, j + 1)], [a_s.iv()])
                stt_(A[:, 0:128], A[:, 130:258], pm[:, 4 + j:5 + j], A[:, 0:128], ALU.mult, ALU.add, [a_s.iv(), psm.iv(4 + j, 5 + j)], [a_s.iv()])
                stt_(A[:, 130:258], A[:, 0:128], 1.0, A[:, 0:128], ALU.mult, ALU.mult, [a_s.iv()], [a_s.iv(), psm.iv(8 + j, 9 + j)],
                     accum=pm[:, 8 + j:9 + j])

        def part3():
            for qb in range(2):
                j = st * 2 + qb
                tile = g * 2 + qb
                a_s = accS4[st][qb]
                stt_(otok.ap[:, tile, h * 128:(h + 1) * 128], a_s.ap[:, 0:128], psm.ap[:, 12 + j:13 + j], gsub.ap, ALU.mult, ALU.mult,
                     [a_s.iv(), psm.iv(12 + j, 13 + j), gsub.iv()], [otok.iv(tile * 512 + h * 128, tile * 512 + h * 128 + 128)])

        return dict(build=build, qk=qk, ex=ex, av=av, copies=copies, part1=part1, part3=part3)

    accS4 = [[carve([128, 264], F32, at=R1 + 16384 + (st_ * 2 + qb_) * 1088) for qb_ in range(2)] for st_ in range(2)]
    psm = carve([128, 16], F32, at=R1 + 20736)

    def part2():
        act(psm.ap[:, 12:16], psm.ap[:, 8:12], AF.Ln, [psm.iv(8, 12)], [psm.iv(12, 16)], bias=EPS, scale=1.0 / 128.0)
        act(psm.ap[:, 12:16], psm.ap[:, 12:16], AF.Exp, [psm.iv(12, 16)], [psm.iv(12, 16)], scale=-0.5)

    pending = None
    for h in range(4):
        for i in range(4):
            A_ = stream_ops(h, 2 * i, 0)
            B_ = stream_ops(h, 2 * i + 1, 1)
            A_["build"]()
            B_["build"]()
            for kb0 in range(2):
                A_["qk"](kb0)
                B_["qk"](kb0)
            if pending is not None:
                pending[0]["part1"]()
                pending[1]["part1"]()
            for kb in range(32):
                for S_ in (A_, B_):
                    S_["ex"](kb)
                    S_["av"](kb)
                    if kb + 2 < 32:
                        S_["qk"](kb + 2)
                if pending is not None and kb == 12:
                    part2()
                if pending is not None and kb == 20:
                    pending[0]["part3"]()
                    pending[1]["part3"]()
            A_["copies"]()
            B_["copies"]()
            pending = (A_, B_)
    pending[0]["part1"]()
    pending[1]["part1"]()
    part2()
    pending[0]["part3"]()
    pending[1]["part3"]()

    accM = [accS4[0][0], accS4[0][1], accS4[1][0], accS4[1][1]]
    msteps = [(qg, mh, j) for qg in range(4) for mh in range(4) for j in range(2)]

    def m_qk(si):
        qg, mh, j = msteps[si]
        jj, r = mh // 2, mh % 2
        q0 = qg * 512
        b = si % 3
        mm(ps[:, b, :], mkT.ap[64 * r:64 * r + 64, jj, j * 128:(j + 1) * 128], mqT.ap[64 * r:64 * r + 64, jj, q0:q0 + 512],
           True, True, [mkT.ivi(jj), mqT.iv(jj * OWN + q0, jj * OWN + q0 + 512)], [PSI(b)])

    def m_ex(si):
        b = si % 3
        pt = PT[0][si % 3]
        act(pt.ap, ps[:, b, :], AF.Exp, [PSI(b)], [pt.iv()])

    def m_av(si):
        qg, mh, j = msteps[si]
        pt = PT[0][si % 3]
        for qb in range(4):
            mm(ps[:, 4 + qb, mh * 66:mh * 66 + 65], pt.ap[:, qb * 128:(qb + 1) * 128], mvaug.ap[:, j, mh, 0:65],
               (mh == 0 and j == 0), (j == 1), [pt.iv(), mvaug.ivi(j)], [PSI(4 + qb, mh * 66, mh * 66 + 65)], skip=True)

    def m_post_copy(qg):
        for qb in range(4):
            cp("dve", accM[qb].ap[:, 0:264], ps[:, 4 + qb, 0:264], [PSI(4 + qb, 0, 264)], [accM[qb].iv()])

    def m_post_math(qg):
        for qb in range(4):
            tile = qg * 4 + qb
            a_ = accM[qb]
            for mh in range(4):
                P.add("dve", lambda e, A=a_.ap, mh=mh: e.reciprocal(out=A[:, mh * 66 + 64:mh * 66 + 65], in_=A[:, mh * 66 + 64:mh * 66 + 65]),
                      reads=[a_.iv()], writes=[a_.iv()])
                ts("dve", otokm.ap[:, tile, mh * 64:(mh + 1) * 64], a_.ap[:, mh * 66:mh * 66 + 64], a_.ap[:, mh * 66 + 64:mh * 66 + 65], None,
                   ALU.mult, None, [a_.iv()], [otokm.ivi(tile)])

    def d_tr(t):
        for cch in range(4):
            tr(psb(3)[:, cch * 128:(cch + 1) * 128], otok.ap[:, t, cch * 128:(cch + 1) * 128], identb.ap,
               [otok.ivi(t), identb.iv()], [PSI(3)])
        evac(oT.ap[:, 0:4, t * 128:(t + 1) * 128], psb(3)[:, 0:512].rearrange("p (a b) -> p a b", a=4),
             [PSI(3)], [oT.iv(0, 4 * OWN)])

    m_qk(0)
    m_qk(1)
    pend = None
    for si in range(32):
        qg, mh, j = msteps[si]
        m_ex(si)
        m_av(si)
        if si + 2 < 32:
            m_qk(si + 2)
        if si % 2 == 0:
            d_tr(si // 2)
        if mh == 3 and j == 1:
            m_post_copy(qg)
            pend = qg
        if pend is not None and (si % 8) == 3:
            m_post_math(pend)
            pend = None
    m_post_math(3)

    if stop == "attn":
        return finish([("otok", otok), ("otokm", otokm)])
    for t in range(16):
        tb = t % 2
        for cch in range(2):
            tr(psb(tb)[:, cch * 128:(cch + 1) * 128], otokm.ap[:, t, cch * 128:(cch + 1) * 128], identb.ap,
               [otokm.ivi(t), identb.iv()], [PSI(tb)])
        evac(oT.ap[:, 6:8, t * 128:(t + 1) * 128], psb(tb)[:, 0:256].rearrange("p (a b) -> p a b", a=2),
             [PSI(tb)], [oT.iv(6 * OWN, 8 * OWN)])

    if stop == "oT":
        return finish([("oT", oT)])
    base = 0
    oh_free = base + 100352
    acc = carve([128, 16, D], F32, at=base)
    h1T = carve([128, 8, OWN], BF16, at=base + 65536)
    gates = carve([128, 16, 32], F32, at=base + 98304)
    LG = carve([128, 16, 36], F32, at=oh_free)
    gw = carve([128, 16, 64], F32, at=R1 + 16384)
    em = carve([128, 16, 32], F32, at=R1 + 20480)
    gw2 = carve([128, 16, 64], F32, at=R1 + 24576)
    woutb = carve([128, 8, D], BF16, at=qT.off)
    gb = carve([128, 2, D], F32, at=Hb.off + 8192)
    gb1 = carve([128, 2, D], F32, at=Hb.off)
    xr = [carve([128, D], F32, at=R1 + 24576 + i * 4096) for i in range(2)]
    zt = [carve([128, D], F32, at=Hst.off + i * 4096) for i in range(1)]
    zt = [carve([128, D], F32, at=ohs.off)]
    h1Tf = carve([128, 8, 128], F32, at=R1)
    wrs = carve([128, 8, 36], F32, at=Hb.off + 16384)
    bcat = carve([128, 36], F32, at=Hb.off + 16384 + 1152)
    assert Hb.off + 16384 + 1152 + 144 <= Hst.off

    dma("pool", woutb.ap, w_out.rearrange("(k p) n -> p k n", p=128), [], [woutb.iv()], lane="wout")
    dma("sp", gb.ap[:, 0, :], ln0_g.partition_broadcast(128), [], [gb.ivi(0)], lane="gbC", group=True)
    dma("sp", gb.ap[:, 1, :], ln0_b.partition_broadcast(128), [], [gb.ivi(1)], lane="gbC", group=True)
    dma("sp", gb1.ap[:, 0, :], ln1_g.partition_broadcast(128), [], [gb1.ivi(0)], lane="gbC", group=True)
    dma("sp", gb1.ap[:, 1, :], ln1_b.partition_broadcast(128), [], [gb1.ivi(1)], lane="gbC", group=True)
    dma("sp", wrs.ap, wr_cat.rearrange("(k p) n -> p k n", p=128), [], [wrs.iv()], lane="gbC", group=True)
    dma("sp", bcat.ap, b_cat.partition_broadcast(128), [], [bcat.iv()], lane="gbC", group=True)
    act(gb.ap, gb.ap, AF.Copy, [gb.iv()], [gb.iv()], scale=ALPHA)
    act(gb1.ap, gb1.ap, AF.Copy, [gb1.iv()], [gb1.iv()], scale=ALPHA)

    xr = xr + [carve([128, D], F32, at=ohs.off)]
    h1Tfs = [h1Tf, carve([128, 8, 128], F32, at=Hst.off)]
    stat1 = carve([128, 16, 16], F32, at=base + 102656)

    def s1_mm(t):
        b0 = 2 * (t % 2)
        for hf in range(2):
            for k in range(8):
                mm(ps[:, b0 + hf, :], oT.ap[:, k, t * 128:(t + 1) * 128], woutb.ap[:, k, hf * 512:(hf + 1) * 512], k == 0, k == 7,
                   [oT.iv(k * OWN + t * 128, k * OWN + t * 128 + 128), woutb.ivi(k)], [PSI(b0 + hf)])

    def s1_front(t):
        xr_ = xr[t % 3]
        dma("sp", xr_.ap, x[t * 128:(t + 1) * 128, :], [], [xr_.iv()], lane="xr%d" % (t % 3))
        act(xr_.ap, xr_.ap, AF.Identity, [xr_.iv(), stat0.iv(2 * t, 2 * t + 2)], [xr_.iv()],
            bias=stat0.ap[:, t, 1:2], scale=stat0.ap[:, t, 0:1])
        tt("dve", xr_.ap, xr_.ap, gb.ap[:, 0, :], ALU.mult, [xr_.iv(), gb.ivi(0)], [xr_.iv()])

    def s1_mid(t):
        xr_ = xr[t % 3]
        tt("pool", xr_.ap, xr_.ap, gb.ap[:, 1, :], ALU.add, [xr_.iv(), gb.ivi(1)], [xr_.iv()])

    def s1_back(t):
        xr_ = xr[t % 3]
        b0 = 2 * (t % 2)
        a_ap = acc.ap[:, t, :]
        tt("dve", a_ap, ps[:, b0:b0 + 2, :].rearrange("p a b -> p (a b)"), xr_.ap, ALU.add, [PSI(b0, 0, 512, nb=2), xr_.iv()], [acc.ivi(t)])
        s1 = stat1.ap[:, t, :]
        P.add("dve", lambda e: e.bn_stats(out=s1[:, 0:6], in_=a_ap[:, 0:512]), reads=[acc.ivi(t)], writes=[stat1.ivi(t)])
        P.add("dve", lambda e: e.bn_stats(out=s1[:, 6:12], in_=a_ap[:, 512:1024]), reads=[acc.ivi(t)], writes=[stat1.ivi(t)])
        P.add("dve", lambda e: e.bn_aggr(out=s1[:, 12:14], in_=s1[:, 0:12]), reads=[stat1.ivi(t)], writes=[stat1.ivi(t)])

    for i in range(18):
        if 0 <= i - 2 < 16:
            s1_back(i - 2)
        if 0 <= i - 1 < 16:
            s1_mid(i - 1)
        if i < 16:
            s1_mm(i)
            s1_front(i)
    act(stat1.ap[:, :, 14], stat1.ap[:, :, 13], AF.Ln, [stat1.iv()], [stat1.iv()], bias=EPS, scale=1.0)
    act(stat1.ap[:, :, 14], stat1.ap[:, :, 14], AF.Exp, [stat1.iv()], [stat1.iv()], scale=-0.5)
    stt_(stat1.ap[:, :, 15], stat1.ap[:, :, 12], -1.0, stat1.ap[:, :, 14], ALU.mult, ALU.mult, [stat1.iv()], [stat1.iv()])
    def s5_T(t):
        bq = 4 + 2 * (t % 2)
        hf_ = h1Tfs[t % 2]
        for k in range(8):
            tr(ps[:, bq + k // 4, (k % 4) * 128:(k % 4 + 1) * 128], acc.ap[:, t, k * 128:(k + 1) * 128], identf.ap,
               [acc.ivi(t), identf.iv()], [PSI(bq, 0, 512, nb=2)])
        for hh in range(2):
            act(hf_.ap[:, 4 * hh:4 * hh + 4, :], ps[:, bq + hh, :].rearrange("p (a b) -> p a b", a=4), AF.Copy,
                [PSI(bq, 0, 512, nb=2)], [hf_.iv(hh * 512, hh * 512 + 512)], scale=1.0 / ALPHA)

    def s5_R(t):
        hf_ = h1Tfs[t % 2]
        bl = t % 2
        for k in range(8):
            mm(ps[:, bl, 0:36], hf_.ap[:, k, :], wrs.ap[:, k, :], k == 0, k == 7, [hf_.ivi(k), wrs.ivi(k)], [PSI(bl, 0, 36)])
        tt("dve", LG.ap[:, t, :], ps[:, bl, 0:36], bcat.ap, ALU.add, [PSI(bl, 0, 36), bcat.iv()], [LG.ivi(t)])

    def s4(t):
        a_ap = acc.ap[:, t, :]
        act(a_ap, a_ap, AF.Identity, [acc.ivi(t), stat1.iv()], [acc.ivi(t)], bias=stat1.ap[:, t, 15:16], scale=stat1.ap[:, t, 14:15])
        tt("dve", a_ap, a_ap, gb1.ap[:, 0, :], ALU.mult, [acc.ivi(t), gb1.ivi(0)], [acc.ivi(t)])
        tt("pool", a_ap, a_ap, gb1.ap[:, 1, :], ALU.add, [acc.ivi(t), gb1.ivi(1)], [acc.ivi(t)])

    for i in range(16 + 3):
        if i < 16:
            s4(i)
        if 0 <= i - 2 < 16:
            s5_T(i - 2)
        if 0 <= i - 3 < 16:
            s5_R(i - 3)

    W = gw.ap
    wiv = gw.iv()
    GLa = LG.ap[:, :, 0:4]
    ELa = LG.ap[:, :, 4:36]
    gmax = W[:, :, 0:1]
    P.add("dve", lambda e: e.tensor_reduce(out=W[:, :, 0], in_=GLa, axis=AX.X, op=ALU.max), reads=[LG.iv()], writes=[wiv])
    tt("dve", W[:, :, 4:8], GLa, gmax.broadcast_to([128, 16, 4]), ALU.is_ge, [LG.iv(), wiv], [wiv])
    tt("dve", W[:, :, 8:12], GLa, gmax.broadcast_to([128, 16, 4]), ALU.subtract, [LG.iv(), wiv], [wiv])
    act(W[:, :, 8:12], W[:, :, 8:12], AF.Exp, [wiv], [wiv])
    P.add("dve", lambda e: e.tensor_reduce(out=W[:, :, 1], in_=W[:, :, 8:12], axis=AX.X, op=ALU.add), reads=[wiv], writes=[wiv])
    P.add("dve", lambda e: e.reciprocal(out=W[:, :, 2], in_=W[:, :, 1]), reads=[wiv], writes=[wiv])
    ts("dve", W[:, :, 12:16], W[:, :, 4:8], -1.0, 1e30, ALU.add, ALU.mult, [wiv], [wiv])
    EM = em.ap
    tt("dve", EM.rearrange("p t (g e) -> p t g e", g=4), ELa.rearrange("p t (g e) -> p t g e", g=4),
       W[:, :, 12:16].unsqueeze(3).broadcast_to([128, 16, 4, 8]), ALU.add, [LG.iv(), wiv], [em.iv()])
    for t in range(16):
        P.add("dve", lambda e, t=t: e.max(out=W[:, t, 16:24], in_=EM[:, t, :]), reads=[em.ivi(t)], writes=[wiv])
    tt("dve", W[:, :, 24], W[:, :, 17], W[:, :, 16], ALU.subtract, [wiv], [wiv])
    act(W[:, :, 25], W[:, :, 24], AF.Exp, [wiv], [wiv])
    ts("dve", W[:, :, 26], W[:, :, 25], 1.0, None, ALU.add, None, [wiv], [wiv])
    P.add("dve", lambda e: e.reciprocal(out=W[:, :, 27], in_=W[:, :, 26]), reads=[wiv], writes=[wiv])
    tt("dve", W[:, :, 27], W[:, :, 27], W[:, :, 2], ALU.mult, [wiv], [wiv])
    tt("dve", W[:, :, 28], W[:, :, 27], W[:, :, 25], ALU.mult, [wiv], [wiv])
    D0 = Hb.off
    gsel = carve([128, 16, 2], F32, at=D0)
    slots_f = carve([128, 32], F32, at=D0 + 128)
    slots_i = carve([128, 32], I32, at=D0 + 256)
    widx_i = carve([128, 192], I32, at=D0 + 512)
    widx_f = carve([128, 192], F32, at=D0 + 77824)
    maskb = carve([128, 16, 32], F32, at=D0 + 1536)
    oh1 = carve([128, 16, 32], F32, at=D0 + 1536 + 2048)
    oh2 = carve([128, 16, 32], F32, at=D0 + 1536 + 4096)
    within = carve([128, 16, 32], F32, at=D0 + 1536 + 6144)
    cntb = carve([128, 16, 32], F32, at=D0 + 1536 + 8192)
    baseb = carve([128, 16, 32], F32, at=D0 + 1536 + 10240)
    slotm = carve([128, 16, 32], F32, at=D0 + 1536 + 12288)
    thr = carve([128, 32, 16], F32, at=D0 + 1536 + 14336)
    tcmp = carve([128, 64, 32], F32, at=D0 + 17920)
    smallr = carve([128, 512], F32, at=D0 + 26112)
    Uc = carve([128, 128], F32, at=D0 + 28160)
    onesc = carve([128, 128], F32, at=D0 + 28672)
    tauc = carve([128, 64], F32, at=D0 + 29184)
    pjc = carve([128, 4], F32, at=D0 + 29440)
    tt("dve", oh1.ap, EM, W[:, :, 16:17].broadcast_to([128, 16, 32]), ALU.is_equal, [em.iv(), wiv], [oh1.iv()])
    tt("dve", oh2.ap, EM, W[:, :, 17:18].broadcast_to([128, 16, 32]), ALU.is_equal, [em.iv(), wiv], [oh2.iv()])
    cp("dve", gsel.ap, W[:, :, 27:29], [wiv], [gsel.iv()])

    if stop == "C":
        return finish([("acc", acc), ("oh1", oh1), ("gsel", gsel)])
    NT = 64
    xs_d = xs_z
    ys_d = nc.dram_tensor("ys_scr", [NT * 128, D], BF16, kind="Internal").ap()
    dma("sp", Uc.ap, u_d, [], [Uc.iv()], lane="rc", group=True)
    dma("sp", onesc.ap, ones_d, [], [onesc.iv()], lane="rc", group=True)
    dma("sp", tauc.ap, tau_d.partition_broadcast(128), [], [tauc.iv()], lane="rc", group=True)
    dma("sp", pjc.ap, pj_d, [], [pjc.iv()], lane="rc", group=True)
    dma("sp", thr.ap, thr_d.partition_broadcast(128), [], [thr.iv()], lane="rc", group=True)
    gb2 = carve([128, 2, D], F32, at=D0 + 61440)
    yo = [carve([128, D], F32, at=D0 + 69632 + i * 4096) for i in range(2)]
    tt("dve", maskb.ap, oh1.ap, oh2.ap, ALU.add, [oh1.iv(), oh2.iv()], [maskb.iv()])
    mflat = maskb.ap.rearrange("p t e -> p (t e)")
    mm(ps[:, 0, :], Uc.ap, mflat, True, True, [Uc.iv(), maskb.iv()], [PSI(0)])
    mm(ps[:, 1, :], onesc.ap, mflat, True, True, [onesc.iv(), maskb.iv()], [PSI(1)])
    cp("dve", within.ap.rearrange("p t e -> p (t e)"), ps[:, 0, :], [PSI(0)], [within.iv()])
    cp("dve", cntb.ap.rearrange("p t e -> p (t e)"), ps[:, 1, :], [PSI(1)], [cntb.iv()])
    memset("dve", baseb.ap[:, 0, :], 0.0, [baseb.ivi(0)])
    for t in range(1, 16):
        tt("dve", baseb.ap[:, t, :], baseb.ap[:, t - 1, :], cntb.ap[:, t - 1, :], ALU.add, [baseb.ivi(t - 1), cntb.ivi(t - 1)], [baseb.ivi(t)])
    R_ = smallr.ap
    riv = smallr.iv()
    tt("dve", R_[:, 0:32], baseb.ap[:, 15, :], cntb.ap[:, 15, :], ALU.add, [baseb.ivi(15), cntb.ivi(15)], [riv])
    tt("dve", tcmp.ap.rearrange("p a b -> p (a b)")[:, 0:512].rearrange("p (e j) -> p e j", e=32),
       R_[:, 0:32].unsqueeze(2).broadcast_to([128, 32, 16]), thr.ap, ALU.is_gt, [riv, thr.iv()], [tcmp.iv()])
    P.add("dve", lambda e: e.tensor_reduce(out=R_[:, 32:64], in_=tcmp.ap.rearrange("p a b -> p (a b)")[:, 0:512].rearrange("p (e j) -> p e j", e=32),
                                           axis=AX.X, op=ALU.add), reads=[tcmp.iv()], writes=[riv])
    memset("dve", R_[:, 64:65], 0.0, [riv])
    for e_ in range(1, 32):
        tt("dve", R_[:, 64 + e_:65 + e_], R_[:, 63 + e_:64 + e_], R_[:, 31 + e_:32 + e_], ALU.add, [riv], [riv])
    ts("dve", R_[:, 96:128], R_[:, 64:96], 128.0, None, ALU.mult, None, [riv], [riv])
    tt("dve", slotm.ap, baseb.ap, within.ap, ALU.add, [baseb.iv(), within.iv()], [slotm.iv()])
    tt("dve", slotm.ap, slotm.ap, R_[:, 96:128].unsqueeze(1).broadcast_to([128, 16, 32]), ALU.add, [slotm.iv(), riv], [slotm.iv()])
    tt("dve", oh1.ap, oh1.ap, slotm.ap, ALU.mult, [oh1.iv(), slotm.iv()], [oh1.iv()])
    tt("dve", oh2.ap, oh2.ap, slotm.ap, ALU.mult, [oh2.iv(), slotm.iv()], [oh2.iv()])
    P.add("dve", lambda e: e.tensor_reduce(out=slots_f.ap[:, 0:16], in_=oh1.ap, axis=AX.X, op=ALU.add), reads=[oh1.iv()], writes=[slots_f.iv()])
    P.add("dve", lambda e: e.tensor_reduce(out=slots_f.ap[:, 16:32], in_=oh2.ap, axis=AX.X, op=ALU.add), reads=[oh2.iv()], writes=[slots_f.iv()])
    cp("dve", slots_i.ap, slots_f.ap, [slots_f.iv()], [slots_i.iv()])
    tt("dve", tcmp.ap, R_[:, 64:96].unsqueeze(1).broadcast_to([128, 64, 32]), tauc.ap.unsqueeze(2).broadcast_to([128, 64, 32]),
       ALU.is_le, [riv, tauc.iv()], [tcmp.iv()])
    P.add("dve", lambda e: e.tensor_reduce(out=R_[:, 128:192], in_=tcmp.ap, axis=AX.X, op=ALU.add), reads=[tcmp.iv()], writes=[riv])
    ts("dve", R_[:, 128:192], R_[:, 128:192], -1.0, 384.0, ALU.add, ALU.mult, [riv], [riv])
    tt("dve", widx_f.ap.rearrange("p (a j) -> p a j", j=3), R_[:, 128:192].unsqueeze(2).broadcast_to([128, 64, 3]),
       pjc.ap[:, 0:3].unsqueeze(1).broadcast_to([128, 64, 3]), ALU.add, [riv, pjc.iv()], [widx_f.iv()])
    ts("dve", widx_f.ap[:, 0:64], R_[:, 128:192], 1.0 / 3.0, pjc.ap[:, 3:4], ALU.mult, ALU.add, [riv, pjc.iv()], [widx_f.iv()])
    cp("dve", widx_i.ap, widx_f.ap, [widx_f.iv()], [widx_i.iv()])
    if stop == "R":
        return finish([("slots_f", slots_f), ("smallr", smallr), ("widx_f", widx_f)])
    h1tok = [carve([128, D + 4], BF16, at=o_) for o_ in (98304, 100416, 103680, D0 + 29696, 90112, 92224, 94336, D0 + 57344)] + \
            [carve([128, D + 4], BF16, at=D0 + 45056 + i * 2112) for i in range(5)] + \
            [carve([128, D + 4], BF16, at=D0 + 78592 + i * 2112) for i in range(3)]
    gpc = carve([128, 32, 4], BF16, at=D0 + 26112 + 1536)
    gtmp = carve([128, 32], F32, at=D0 + 26112 + 1536 + 256)
    gflat = gsel.ap.rearrange("p t k -> p (t k)")
    memset("dve", gpc.ap, 0.0, [gpc.iv()])
    cp("dve", gpc.ap[:, :, 0], gflat, [gsel.iv()], [gpc.iv()])
    tt("dve", gtmp.ap, gflat, gpc.ap[:, :, 0], ALU.subtract, [gsel.iv(), gpc.iv()], [gtmp.iv()])
    cp("dve", gpc.ap[:, :, 1], gtmp.ap, [gtmp.iv()], [gpc.iv()])
    tt("dve", gtmp.ap, gtmp.ap, gpc.ap[:, :, 1], ALU.subtract, [gtmp.iv(), gpc.iv()], [gtmp.iv()])
    cp("dve", gpc.ap[:, :, 2], gtmp.ap, [gtmp.iv()], [gpc.iv()])
    XSW = ("xs", 0, 1)
    YSW = ("ys", 0, 1)
    for t in range(16):
        for k in range(2):
            hb_ = h1tok[(t % 8) * 2 + k]
            col = k * 16 + t
            act(hb_.ap[:, 0:D], acc.ap[:, t, :], AF.Copy, [acc.ivi(t)], [hb_.iv(0, D)], scale=1.0 / ALPHA)
            cp("dve", hb_.ap[:, D:D + 4], gpc.ap[:, t * 2 + k, :], [gpc.iv()], [hb_.iv(D, D + 4)])
            P.add("pool", lambda e, hb_=hb_, col=col: e.indirect_dma_start(
                out=xs_d, out_offset=bass.IndirectOffsetOnAxis(ap=slots_i.ap[:, col:col + 1].bitcast(U32), axis=0),
                in_=hb_.ap, in_offset=None), reads=[hb_.iv(), slots_i.iv()], writes=[("xs", 2 + col, 3 + col)], lane="sc%d" % ((t % 8) * 2 + k))
    wbuf = [carve([128, 6144], BF16, at=o_) for o_ in (65536, 77824, D0 + 45056, D0 + 78592)]
    NWB = len(wbuf)
    assert D0 + 78592 + 12288 <= SBUF_BYTES
    wb_rows = wb_d.rearrange("(r j) n -> r (j n)", j=3)
    xst = [carve([128, D + 4], BF16, at=90112), carve([128, D + 4], BF16, at=D0 + 57344)]
    gcol = smallr.ap[:, 320:324]
    XsT = [carve([128, 8, 128], BF16, at=94208 + i * 2048) for i in range(2)]
    sa = [carve([128, 256], BF16, at=D0 + 33792 + i * 512) for i in range(2)]
    hid = [carve([128, 256], BF16, at=D0 + 34816 + i * 512) for i in range(2)]
    hidT = [carve([128, 2, 128], BF16, at=D0 + 35840 + i * 512) for i in range(2)]
    ysb = [carve([128, D], BF16, at=D0 + 36864 + i * 4096) for i in range(2)]
    allxs = [("xs", 0, 64)]
    def a_T(tau):
        q = tau % 2
        wb_ = wbuf[tau % NWB]
        P.add("pool", lambda e, wb_=wb_, tau=tau: e.indirect_dma_start(
            out=wb_.ap, out_offset=None, in_=wb_rows,
            in_offset=bass.IndirectOffsetOnAxis(ap=widx_i.ap[:, tau:tau + 1].bitcast(U32), axis=0)),
            reads=[widx_i.iv(), ("wb", 0, 128)], writes=[wb_.iv()], lane="wg%d" % (tau % NWB))
        if tau == 0:
            for t2 in range(2):
                dma("sp", xst[t2].ap, xs_d[t2 * 128:(t2 + 1) * 128, :], allxs, [xst[t2].iv()], lane="xst%d" % t2)
        for k in range(8):
            tr(psb(0)[:, k * 128:(k + 1) * 128], xst[q].ap[:, k * 128:(k + 1) * 128], identb.ap, [xst[q].iv(), identb.iv()], [PSI(0)])
        P.add("dve", lambda e, q=q: e.tensor_reduce(out=gcol[:, q:q + 1], in_=xst[q].ap[:, D:D + 3], axis=AX.X, op=ALU.add),
              reads=[xst[q].iv()], writes=[smallr.iv(320 + q, 321 + q)])
        if tau + 2 < NT:
            dma("sp", xst[q].ap, xs_d[(tau + 2) * 128:(tau + 3) * 128, :], allxs, [xst[q].iv()], lane="xst%d" % q)
        evac(XsT[q].ap.rearrange("p a b -> p (a b)"), psb(0), [PSI(0)], [XsT[q].iv()])

    def a_up(tau):
        q = tau % 2
        wb_ = wbuf[tau % NWB]
        for k in range(8):
            mm(ps[:, 2 + q, :], XsT[q].ap[:, k, :], wb_.ap[:, k * 512:(k + 1) * 512], k == 0, k == 7,
               [XsT[q].ivi(k), wb_.iv(k * 512, (k + 1) * 512)], [PSI(2 + q)])
        act(sa[q].ap, ps[:, 2 + q, 0:256], AF.Silu, [PSI(2 + q)], [sa[q].iv()])
        tt("dve", hid[q].ap, sa[q].ap, ps[:, 2 + q, 256:512], ALU.mult, [sa[q].iv(), PSI(2 + q)], [hid[q].iv()])

    def b_(tau):
        q = tau % 2
        wb_ = wbuf[tau % NWB]
        for c in range(2):
            tr(psb(1)[:, c * 128:(c + 1) * 128], hid[q].ap[:, c * 128:(c + 1) * 128], identb.ap, [hid[q].iv(), identb.iv()], [PSI(1)])
        evac(hidT[q].ap.rearrange("p a b -> p (a b)"), psb(1)[:, 0:256], [PSI(1)], [hidT[q].iv()])
        for hf in range(2):
            for c in range(2):
                mm(ps[:, 4 + 2 * q + hf, :], hidT[q].ap[:, c, :], wb_.ap[:, 4096 + c * 1024 + hf * 512:4096 + c * 1024 + hf * 512 + 512], c == 0, c == 1,
                   [hidT[q].ivi(c), wb_.iv(4096, 6144)], [PSI(4 + 2 * q + hf)])
        act(ysb[q].ap[:, 0:512], ps[:, 4 + 2 * q, :], AF.Identity, [PSI(4 + 2 * q), smallr.iv(320 + q, 321 + q)], [ysb[q].iv(0, 512)], scale=gcol[:, q:q + 1])
        ts("dve", ysb[q].ap[:, 512:1024], ps[:, 5 + 2 * q, :], gcol[:, q:q + 1], None, ALU.mult, None, [PSI(5 + 2 * q), smallr.iv(320 + q, 321 + q)], [ysb[q].iv(512, 1024)])
        dma("sp", ys_d[tau * 128:(tau + 1) * 128, :], ysb[q].ap, [ysb[q].iv()], [("ys", 2 + tau, 3 + tau)], lane="ysw%d" % q)

    a_T(0)
    a_up(0)
    for tau in range(NT):
        if tau + 1 < NT:
            a_T(tau + 1)
        b_(tau)
        if tau + 1 < NT:
            a_up(tau + 1)
    dma("sp", gb2.ap[:, 0, :], ln2_g.partition_broadcast(128), [], [gb2.ivi(0)], lane="gbE", group=True)
    dma("sp", gb2.ap[:, 1, :], ln2_b.partition_broadcast(128), [], [gb2.ivi(1)], lane="gbE", group=True)
    if stop == "D":
        return finish([("acc", acc)])
    allys = [("ys", 0, 128)]
    for k in range(2):
        for t in range(16):
            a_ap = acc.ap[:, t, :]
            col = k * 16 + t
            P.add("pool", lambda e, a_ap=a_ap, col=col: e.indirect_dma_start(
                out=a_ap, out_offset=None, in_=ys_d,
                in_offset=bass.IndirectOffsetOnAxis(ap=slots_i.ap[:, col:col + 1].bitcast(U32), axis=0), compute_op=ALU.add),
                reads=[slots_i.iv(), acc.ivi(t)] + allys, writes=[acc.ivi(t)], lane="yg%d" % t)
    for t in range(16):
        a_ap = acc.ap[:, t, :]
        s1 = stat1.ap[:, t, :]
        P.add("dve", lambda e, s1=s1, a_ap=a_ap: e.bn_stats(out=s1[:, 0:6], in_=a_ap[:, 0:512]), reads=[acc.ivi(t)], writes=[stat1.ivi(t)])
        P.add("dve", lambda e, s1=s1, a_ap=a_ap: e.bn_stats(out=s1[:, 6:12], in_=a_ap[:, 512:1024]), reads=[acc.ivi(t)], writes=[stat1.ivi(t)])
        P.add("dve", lambda e, s1=s1: e.bn_aggr(out=s1[:, 12:14], in_=s1[:, 0:12]), reads=[stat1.ivi(t)], writes=[stat1.ivi(t)])
    act(stat1.ap[:, :, 14], stat1.ap[:, :, 13], AF.Ln, [stat1.iv()], [stat1.iv()], bias=EPS, scale=1.0)
    act(stat1.ap[:, :, 14], stat1.ap[:, :, 14], AF.Exp, [stat1.iv()], [stat1.iv()], scale=-0.5)
    stt_(stat1.ap[:, :, 15], stat1.ap[:, :, 12], -1.0, stat1.ap[:, :, 14], ALU.mult, ALU.mult, [stat1.iv()], [stat1.iv()])
    yo = yo + [carve([128, D], F32, at=D0 + 36864 + i * 4096) for i in range(2)]
    for t in range(16):
        a_ap = acc.ap[:, t, :]
        yo_ = yo[t % 4]
        act(yo_.ap, a_ap, AF.Identity, [acc.ivi(t), stat1.iv()], [yo_.iv()], bias=stat1.ap[:, t, 15:16], scale=stat1.ap[:, t, 14:15])
        for eng_, c0_, c1_ in (("dve", 0, 576), ("pool", 576, 1024)):
            tt(eng_, yo_.ap[:, c0_:c1_], yo_.ap[:, c0_:c1_], gb2.ap[:, 0, c0_:c1_], ALU.mult, [yo_.iv(c0_, c1_), gb2.ivi(0)], [yo_.iv(c0_, c1_)])
            tt(eng_, yo_.ap[:, c0_:c1_], yo_.ap[:, c0_:c1_], gb2.ap[:, 1, c0_:c1_], ALU.add, [yo_.iv(c0_, c1_), gb2.ivi(1)], [yo_.iv(c0_, c1_)])
        dma("sp", y[t * 128:(t + 1) * 128, :], yo_.ap, [yo_.iv()], [("y", t, t + 1)], lane="yo%d" % (t % 4))

    P.emit()
    return nc


def _pack_experts(w1, w3, w2):
    a = np.concatenate([w1.reshape(NE, 8, 128, 256), w3.reshape(NE, 8, 128, 256)], axis=3)
    a = a.transpose(0, 2, 1, 3).reshape(NE, 128, 4096)
    b2 = w2.reshape(NE, 2, 128, D).transpose(0, 2, 1, 3).reshape(NE, 128, 2048)
    return np.ascontiguousarray(np.concatenate([a, b2], axis=2).reshape(NE * 128 * 3, 2048))


_NC = None


def kernel(x, mem, ln0_g, ln0_b, rel_bias, w_in, w_mem_kv, w_fourier, lambda_q1, lambda_k1, lambda_q2, lambda_k2,
           subln_g, w_out, ln1_g, ln1_b, w_group, b_group, w_router, b_router, w1, w3, w2, ln2_g, ln2_b):
    global _NC
    f = lambda a: np.ascontiguousarray(np.asarray(a, dtype=np.float32))
    x = f(x)
    mem = f(mem)
    shared = {
        "g0T": f(f(ln0_g).reshape(8, 128).T), "b0T": f(f(ln0_b).reshape(8, 128).T),
        "ln0_g": f(ln0_g), "ln0_b": f(ln0_b), "rel_bias": f(rel_bias), "w_in": f(w_in)[0], "w_mem": f(w_mem_kv)[0],
        "w_four": f(w_fourier)[0], "lq1": f(lambda_q1)[0], "lk1": f(lambda_k1)[0], "lq2": f(lambda_q2)[0], "lk2": f(lambda_k2)[0],
        "subln": f(subln_g)[0], "w_out": f(w_out)[0], "ln1_g": f(ln1_g)[0], "ln1_b": f(ln1_b)[0],
        "wr_cat": f(np.concatenate([f(w_group)[0], f(w_router)[0]], axis=1)),
        "b_cat": f(np.concatenate([f(b_group)[0], f(b_router)[0]], axis=0)),
        "wp": _pack_experts(f(w1)[0], f(w3)[0], f(w2)[0]), "ln2_g": f(ln2_g)[0], "ln2_b": f(ln2_b)[0],
    }
    consts = [_host_consts(0), _host_consts(1)]
    in_maps = []
    for c in range(8):
        b, half = c // 2, c % 2
        m = dict(shared)
        m["x"] = np.ascontiguousarray(np.roll(x[b], -OWN * half, axis=0))
        m["mem"] = mem[b]
        m.update(consts[half])
        in_maps.append(m)
    if _NC is None:
        _NC = build_nc()
    res = run_bass_kernel_spmd(_NC, in_maps, core_ids=list(range(8)))
    out = np.empty((4, S, D), np.float32)
    for c in range(8):
        b, half = c // 2, c % 2
        out[b, half * OWN:(half + 1) * OWN, :] = res.results[c]["y"]
    return out
```
